# Optimizing a Trainium2 kernel written in Bass

```python
import math
import jax, jax.numpy as jnp
from jax import lax
import numpy as np

D_MODEL = 1024
BATCH = 4
SEQ = 4096
DEPTH = 4

CHUNK = 64
N_MIXERS = 3
N_LAYERS_A = (DEPTH + 2) // 3
N_LAYERS_B = (DEPTH + 1) // 3
N_LAYERS_C = DEPTH // 3

DEEPNORM_ALPHA = (2.0 * DEPTH) ** 0.25
DEEPNORM_BETA = (8.0 * DEPTH) ** -0.25
LN_EPS = 1e-5
RMS_EPS = 1e-6

MLA_HEADS = 8
MLA_NOPE = 128
MLA_ROPE = 64
MLA_V = 128
MLA_Q_LORA = 384
MLA_KV_LORA = 256
MLA_DOWN = MLA_Q_LORA + MLA_KV_LORA + MLA_ROPE
ROPE_THETA = 10000.0
Q_BLOCK = 128

POOL_WINDOWS = (2, 4, 8, 16)
POOL_GROUPS = len(POOL_WINDOWS)
POOL_GROUP_DIM = D_MODEL // POOL_GROUPS

GDN_QK_HEADS = 8
GDN_V_HEADS = 16
GDN_DK = 128
GDN_DV = 128
GDN_CONV = 4
GDN_Q_DIM = GDN_QK_HEADS * GDN_DK
GDN_V_DIM = GDN_V_HEADS * GDN_DV
GDN_CONV_DIM = 2 * GDN_Q_DIM + GDN_V_DIM
GDN_PROJ_DIM = GDN_CONV_DIM + GDN_V_DIM + 2 * GDN_V_HEADS

N_EXPERTS = 32
TOP_K = 4
D_EXPERT = D_MODEL
SWIGLU_LIMIT = 7.0
SWIGLU_ALPHA = 1.702
EXPERT_BLOCK = 128

kernel_name = "hybrid_mla_pool_gdn_moe_deepnorm_adaln"


def layer_norm(x, g, b):
    xf = x.astype(jnp.float32)
    mu = jnp.mean(xf, axis=-1, keepdims=True)
    var = jnp.mean(jnp.square(xf - mu), axis=-1, keepdims=True)
    return ((xf - mu) * lax.rsqrt(var + LN_EPS) * g + b).astype(x.dtype)


def rms_norm(x, g):
    xf = x.astype(jnp.float32)
    return (xf * lax.rsqrt(jnp.mean(jnp.square(xf), axis=-1, keepdims=True) + RMS_EPS) * g).astype(x.dtype)


def l2_normalize(x):
    return x * lax.rsqrt(jnp.sum(jnp.square(x), axis=-1, keepdims=True) + RMS_EPS)


def rope_tables(seq_len):
    pos = jnp.arange(seq_len, dtype=jnp.float32)
    inv_freq = ROPE_THETA ** (-jnp.arange(0, MLA_ROPE, 2, dtype=jnp.float32) / MLA_ROPE)
    ang = pos[:, None] * inv_freq[None, :]
    return jnp.cos(ang), jnp.sin(ang)


def apply_rotary(x, cos, sin):
    half = x.shape[-1] // 2
    x1, x2 = x[..., :half].astype(jnp.float32), x[..., half:].astype(jnp.float32)
    return jnp.concatenate([x1 * cos - x2 * sin, x2 * cos + x1 * sin], axis=-1).astype(x.dtype)


def mla_mixer(h, cos, sin, w_dqkv, g_q, g_kv, w_uq, w_ukv, w_o):
    B, S, _ = h.shape
    down = h @ w_dqkv
    cq, ckv, k_rope = jnp.split(down, [MLA_Q_LORA, MLA_Q_LORA + MLA_KV_LORA], axis=-1)
    q = (rms_norm(cq, g_q) @ w_uq).reshape(B, S, MLA_HEADS, MLA_NOPE + MLA_ROPE)
    q_nope = q[..., :MLA_NOPE]
    q_rope = apply_rotary(q[..., MLA_NOPE:], cos[None, :, None, :], sin[None, :, None, :])
    k_rope = apply_rotary(k_rope, cos[None], sin[None])
    kv = (rms_norm(ckv, g_kv) @ w_ukv).reshape(B, S, MLA_HEADS, MLA_NOPE + MLA_V)
    k_nope, v = kv[..., :MLA_NOPE], kv[..., MLA_NOPE:]
    scale = (MLA_NOPE + MLA_ROPE) ** -0.5
    n_blk = S // Q_BLOCK
    qn_b = q_nope.reshape(B, n_blk, Q_BLOCK, MLA_HEADS, MLA_NOPE).swapaxes(0, 1)
    qr_b = q_rope.reshape(B, n_blk, Q_BLOCK, MLA_HEADS, MLA_ROPE).swapaxes(0, 1)
    key_chunk = jnp.arange(S) // CHUNK

    def attend(args):
        qn, qr, blk = args
        s = (jnp.einsum('bqhd,bkhd->bhqk', qn, k_nope, preferred_element_type=jnp.float32)
             + jnp.einsum('bqhr,bkr->bhqk', qr, k_rope, preferred_element_type=jnp.float32)) * scale
        q_chunk = (blk * Q_BLOCK + jnp.arange(Q_BLOCK)) // CHUNK
        mask = key_chunk[None, :] <= q_chunk[:, None]
        s = jnp.where(mask, s, -jnp.inf)
        p = jax.nn.softmax(s, axis=-1).astype(v.dtype)
        return jnp.einsum('bhqk,bkhd->bqhd', p, v)

    o = lax.map(attend, (qn_b, qr_b, jnp.arange(n_blk)))
    o = o.swapaxes(0, 1).reshape(B, S, MLA_HEADS * MLA_V)
    return o @ w_o


def pool_mixer(h, w_pool, ch_scale):
    B, S, D = h.shape
    hf = h.astype(jnp.float32)
    cs = jnp.concatenate([jnp.zeros((B, 1, D), jnp.float32), jnp.cumsum(hf, axis=1)], axis=1)
    hi = jnp.arange(1, S + 1)
    upper = cs[:, 1:]
    groups = []
    for g, win in enumerate(POOL_WINDOWS):
        sl = slice(g * POOL_GROUP_DIM, (g + 1) * POOL_GROUP_DIM)
        lo = jnp.maximum(hi - win, 0)
        count = (hi - lo).astype(jnp.float32)[None, :, None]
        mean = (upper[:, :, sl] - cs[:, lo, sl]) / count
        groups.append(mean - hf[:, :, sl])
    d = jnp.stack(groups, axis=2).astype(h.dtype)
    y = jnp.einsum('bsgc,gcd->bsgd', d, w_pool).reshape(B, S, D)
    return y * ch_scale


def causal_depthwise_conv(x, w):
    k_w, ch = w.shape
    return lax.conv_general_dilated(x, w[:, None, :], window_strides=(1,), padding=((k_w - 1, 0),),
                                    dimension_numbers=('NWC', 'WIO', 'NWC'), feature_group_count=ch)


def gated_delta_rule_chunked(q, k, v, beta, g):
    B, S, H, DK = q.shape
    DV = v.shape[-1]
    N = S // CHUNK

    def to_chunks(t):
        t = t.reshape(B, N, CHUNK, H, *t.shape[3:])
        return jnp.moveaxis(t, 3, 1)

    q, k, v, beta, g = (to_chunks(t) for t in (q, k, v, beta, g))
    G = jnp.cumsum(g, axis=-1)
    tril_incl = jnp.tril(jnp.ones((CHUNK, CHUNK), bool))
    tril_strict = jnp.tril(jnp.ones((CHUNK, CHUNK), bool), -1)
    decay = jnp.exp(jnp.where(tril_incl, G[..., :, None] - G[..., None, :], -jnp.inf))
    k_beta = k * beta[..., None]
    v_beta = v * beta[..., None]
    m = jnp.where(tril_strict, jnp.einsum('bhnid,bhnjd->bhnij', k_beta, k) * decay, 0.0)
    eye = jnp.eye(CHUNK, dtype=jnp.float32)
    t_inv = lax.linalg.triangular_solve(eye + m, jnp.broadcast_to(eye, m.shape), left_side=True,
                                        lower=True, unit_diagonal=True)
    u = t_inv @ v_beta
    w = t_inv @ (k_beta * jnp.exp(G)[..., None])
    qg = q * jnp.exp(G)[..., None]
    a_intra = jnp.einsum('bhnid,bhnjd->bhnij', q, k) * decay
    kd = k * jnp.exp(G[..., -1:] - G)[..., None]
    g_last = jnp.exp(G[..., -1])

    def step(state, xs):
        qg_n, w_n, u_n, a_n, kd_n, gl_n = xs
        v_new = u_n - jnp.einsum('bhcd,bhde->bhce', w_n, state)
        o_n = jnp.einsum('bhcd,bhde->bhce', qg_n, state) + jnp.einsum('bhij,bhje->bhie', a_n, v_new)
        state = state * gl_n[..., None, None] + jnp.einsum('bhcd,bhce->bhde', kd_n, v_new)
        return state, o_n

    xs = tuple(jnp.moveaxis(t, 2, 0) for t in (qg, w, u, a_intra, kd, g_last))
    s0 = jnp.zeros((B, H, DK, DV), jnp.float32)
    _, o = lax.scan(step, s0, xs)
    return jnp.transpose(o, (1, 0, 3, 2, 4)).reshape(B, S, H, DV)


def gdn_mixer(h, w_in, w_conv, a_log, dt_bias, g_norm, w_o):
    B, S, _ = h.shape
    proj = h @ w_in
    qkv, z, b_raw, a_raw = jnp.split(
        proj, [GDN_CONV_DIM, GDN_CONV_DIM + GDN_V_DIM, GDN_CONV_DIM + GDN_V_DIM + GDN_V_HEADS], axis=-1)
    qkv = jax.nn.silu(causal_depthwise_conv(qkv, w_conv))
    q, k, v = jnp.split(qkv, [GDN_Q_DIM, 2 * GDN_Q_DIM], axis=-1)
    rep = GDN_V_HEADS // GDN_QK_HEADS
    q = jnp.repeat(l2_normalize(q.reshape(B, S, GDN_QK_HEADS, GDN_DK).astype(jnp.float32)), rep, axis=2)
    k = jnp.repeat(l2_normalize(k.reshape(B, S, GDN_QK_HEADS, GDN_DK).astype(jnp.float32)), rep, axis=2)
    v = v.reshape(B, S, GDN_V_HEADS, GDN_DV).astype(jnp.float32)
    beta = jax.nn.sigmoid(b_raw.astype(jnp.float32))
    g = -jnp.exp(a_log.astype(jnp.float32)) * jax.nn.softplus(a_raw.astype(jnp.float32) + dt_bias)
    o = gated_delta_rule_chunked(q * GDN_DK ** -0.5, k, v, beta, g)
    o = rms_norm(o, g_norm) * jax.nn.silu(z.reshape(B, S, GDN_V_HEADS, GDN_DV).astype(jnp.float32))
    return o.reshape(B, S, GDN_V_DIM).astype(h.dtype) @ w_o


def clamped_swiglu(gu):
    gate, up = jnp.split(gu, 2, axis=-1)
    gate = jnp.minimum(gate, SWIGLU_LIMIT)
    up = jnp.clip(up, -SWIGLU_LIMIT, SWIGLU_LIMIT)
    return gate * jax.nn.sigmoid(SWIGLU_ALPHA * gate) * (up + 1.0)


def moe_ffn(h, w_router, b_router, w_gate_up, b_gate_up, w_down, b_down):
    B, S, D = h.shape
    T = B * S
    xt = h.reshape(T, D)
    logits = (xt @ w_router).astype(jnp.float32) + b_router
    top_vals, top_idx = lax.top_k(logits, TOP_K)
    gates = jax.nn.softmax(top_vals, axis=-1)
    n_assign = T * TOP_K
    e_flat = top_idx.reshape(-1)
    tok_flat = jnp.arange(n_assign, dtype=jnp.int32) // TOP_K
    gate_flat = gates.reshape(-1)
    order = jnp.argsort(e_flat)
    e_sorted = e_flat[order]
    counts = jnp.bincount(e_flat, length=N_EXPERTS)
    padded = (counts + EXPERT_BLOCK - 1) // EXPERT_BLOCK * EXPERT_BLOCK
    pend = jnp.cumsum(padded)
    pstart = pend - padded
    sstart = jnp.cumsum(counts) - counts
    dest = pstart[e_sorted] + jnp.arange(n_assign, dtype=jnp.int32) - sstart[e_sorted]
    n_rows = -(-n_assign // EXPERT_BLOCK) * EXPERT_BLOCK + N_EXPERTS * EXPERT_BLOCK
    n_blocks = n_rows // EXPERT_BLOCK
    row_tok = jnp.full((n_rows,), T, jnp.int32).at[dest].set(tok_flat[order])
    row_gate = jnp.zeros((n_rows,), jnp.float32).at[dest].set(gate_flat[order])
    blk_expert = jnp.minimum(
        jnp.searchsorted(pend, jnp.arange(n_blocks, dtype=pend.dtype) * EXPERT_BLOCK, side='right'),
        N_EXPERTS - 1)
    x_rows = jnp.concatenate([xt, jnp.zeros((1, D), xt.dtype)], axis=0)[row_tok]
    x_rows = x_rows.reshape(n_blocks, EXPERT_BLOCK, D)

    def expert_block(args):
        xb, e = args
        gu = xb @ w_gate_up[e] + b_gate_up[e]
        return clamped_swiglu(gu) @ w_down[e] + b_down[e]

    y = lax.map(expert_block, (x_rows, blk_expert)).reshape(n_rows, D)
    out = jnp.zeros((T + 1, D), jnp.float32).at[row_tok].add(y.astype(jnp.float32) * row_gate[:, None])
    return out[:T].reshape(B, S, D).astype(h.dtype)


def setup_inputs(seed: int = 0) -> dict:
    key = jax.random.key(seed)
    ks = iter(jax.random.split(key, 40))

    def nrm(shape, s):
        return jax.random.normal(next(ks), shape, jnp.float32) * s

    D = D_MODEL
    dt = jnp.exp(jax.random.uniform(next(ks), (N_LAYERS_C, GDN_V_HEADS), jnp.float32,
                                    math.log(1e-3), math.log(1e-1)))
    return {
        "x": nrm((BATCH, SEQ, D), 1.0),
        "c": nrm((BATCH, D), 1.0),
        "ada_w": nrm((DEPTH, D, 6 * D), 0.25 * D ** -0.5),
        "ada_b": nrm((DEPTH, 6 * D), 0.01),
        "ln_g": 1.0 + nrm((DEPTH, 2, D), 0.05),
        "ln_b": nrm((DEPTH, 2, D), 0.02),
        "mla_w_dqkv": nrm((N_LAYERS_A, D, MLA_DOWN), D ** -0.5),
        "mla_g_q": 1.0 + nrm((N_LAYERS_A, MLA_Q_LORA), 0.05),
        "mla_g_kv": 1.0 + nrm((N_LAYERS_A, MLA_KV_LORA), 0.05),
        "mla_w_uq": nrm((N_LAYERS_A, MLA_Q_LORA, MLA_HEADS * (MLA_NOPE + MLA_ROPE)), MLA_Q_LORA ** -0.5),
        "mla_w_ukv": nrm((N_LAYERS_A, MLA_KV_LORA, MLA_HEADS * (MLA_NOPE + MLA_V)), MLA_KV_LORA ** -0.5),
        "mla_w_o": nrm((N_LAYERS_A, MLA_HEADS * MLA_V, D), (MLA_HEADS * MLA_V) ** -0.5 * DEEPNORM_BETA),
        "pool_w": nrm((N_LAYERS_B, POOL_GROUPS, POOL_GROUP_DIM, POOL_GROUP_DIM),
                       POOL_GROUP_DIM ** -0.5 * DEEPNORM_BETA),
        "pool_scale": 1.0 + nrm((N_LAYERS_B, D), 0.05),
        "gdn_w_in": nrm((N_LAYERS_C, D, GDN_PROJ_DIM), D ** -0.5),
        "gdn_w_conv": nrm((N_LAYERS_C, GDN_CONV, GDN_CONV_DIM), GDN_CONV ** -0.5),
        "gdn_a_log": jnp.log(jax.random.uniform(next(ks), (N_LAYERS_C, GDN_V_HEADS), jnp.float32, 1.0, 16.0)),
        "gdn_dt_bias": dt + jnp.log(-jnp.expm1(-dt)),
        "gdn_g_norm": 1.0 + nrm((N_LAYERS_C, GDN_DV), 0.05),
        "gdn_w_o": nrm((N_LAYERS_C, GDN_V_DIM, D), GDN_V_DIM ** -0.5 * DEEPNORM_BETA),
        "moe_w_router": nrm((DEPTH, D, N_EXPERTS), D ** -0.5),
        "moe_b_router": nrm((DEPTH, N_EXPERTS), 0.01),
        "moe_w_gate_up": nrm((DEPTH, N_EXPERTS, D, 2 * D_EXPERT), D ** -0.5),
        "moe_b_gate_up": nrm((DEPTH, N_EXPERTS, 2 * D_EXPERT), 0.01),
        "moe_w_down": nrm((DEPTH, N_EXPERTS, D_EXPERT, D), D_EXPERT ** -0.5 * DEEPNORM_BETA),
        "moe_b_down": nrm((DEPTH, N_EXPERTS, D), 0.01),
    }


def reference(x, c, ada_w, ada_b, ln_g, ln_b,
              mla_w_dqkv, mla_g_q, mla_g_kv, mla_w_uq, mla_w_ukv, mla_w_o,
              pool_w, pool_scale,
              gdn_w_in, gdn_w_conv, gdn_a_log, gdn_dt_bias, gdn_g_norm, gdn_w_o,
              moe_w_router, moe_b_router, moe_w_gate_up, moe_b_gate_up, moe_w_down, moe_b_down):
    S = x.shape[1]
    cos, sin = rope_tables(S)
    mods = jnp.einsum('bd,lde->lbe', jax.nn.silu(c), ada_w) + ada_b[:, None, :]
    for i in range(DEPTH):
        shift_t, scale_t, gate_t, shift_f, scale_f, gate_f = jnp.split(mods[i][:, None, :], 6, axis=-1)
        kind, j = i % N_MIXERS, i // N_MIXERS
        h = x * (1.0 + scale_t) + shift_t
        if kind == 0:
            y = mla_mixer(h, cos, sin, mla_w_dqkv[j], mla_g_q[j], mla_g_kv[j], mla_w_uq[j], mla_w_ukv[j], mla_w_o[j])
        elif kind == 1:
            y = pool_mixer(h, pool_w[j], pool_scale[j])
        else:
            y = gdn_mixer(h, gdn_w_in[j], gdn_w_conv[j], gdn_a_log[j], gdn_dt_bias[j], gdn_g_norm[j], gdn_w_o[j])
        x = layer_norm(DEEPNORM_ALPHA * x + (1.0 + gate_t) * y, ln_g[i, 0], ln_b[i, 0])
        h = x * (1.0 + scale_f) + shift_f
        y = moe_ffn(h, moe_w_router[i], moe_b_router[i], moe_w_gate_up[i], moe_b_gate_up[i],
                    moe_w_down[i], moe_b_down[i])
        x = layer_norm(DEEPNORM_ALPHA * x + (1.0 + gate_f) * y, ln_g[i, 1], ln_b[i, 1])
    return x
```

```python
import numpy as np
import ml_dtypes
import concourse.bass as bass
import concourse.mybir as mybir
from concourse.bass_utils import run_bass_kernel_spmd

F32 = mybir.dt.float32
BF16 = mybir.dt.bfloat16
AF = mybir.ActivationFunctionType
ALU = mybir.AluOpType
AX = mybir.AxisListType
ENGS = ["pe", "act", "dve", "pool", "sp"]
IN, OUT = "ExternalInput", "ExternalOutput"

D = 1024
ALPHA = 8.0 ** 0.25
LN_EPS = 1e-5


class Prog:
    def __init__(self, n_dma_sems=8, same_engine_sync=True):
        self.nc = bass.Bass("TRN2", target_bir_lowering=False)
        self.same_engine_sync = same_engine_sync
        self.lists = {e: [] for e in ENGS}
        self.cnt = {e: 0 for e in ENGS}
        self.waited = {e: {} for e in ENGS}
        self.last_w = {}
        self.last_r = {}
        self.n_dma_sems = n_dma_sems
        self.dma_val = {}
        self.dma_rr = {e: 0 for e in ENGS}
        self._ctx = []
        self.sems = {}
        self.out_waits = []
        self.ninstr = 0

    def enter(self, cm):
        v = cm.__enter__()
        self._ctx.append(cm)
        return v

    def mark(self):
        return len(self._ctx)

    def release(self, mark):
        while len(self._ctx) > mark:
            self._ctx.pop().__exit__(None, None, None)

    def sbuf(self, name, shape, dtype):
        return self.enter(self.nc.sbuf_tensor("sb_" + name, list(shape), dtype))

    def psum(self, name, shape, dtype):
        return self.enter(self.nc.psum_tensor("pp_" + name, list(shape), dtype))

    def dram(self, name, shape, dtype, kind):
        return self.nc.dram_tensor(name, list(shape), dtype, kind=kind).ap()

    def _need(self, eng, key, val, waits):
        if val <= 0:
            return
        if key == eng and (not self.same_engine_sync or eng == "pe" or val > self.cnt[eng]):
            return
        if self.waited[eng].get(key, 0) >= val:
            return
        self.waited[eng][key] = val
        waits.append((key, val))

    def _deps(self, eng, reads, writes):
        waits = []
        for t in reads:
            lw = self.last_w.get(t)
            if lw is not None:
                self._need(eng, lw[0], lw[1], waits)
        for t in writes:
            lw = self.last_w.get(t)
            if lw is not None:
                self._need(eng, lw[0], lw[1], waits)
            for k, v in self.last_r.get(t, {}).items():
                if k == eng:
                    continue
                self._need(eng, k, v, waits)
        return waits

    def _mark(self, key, val, reads, writes):
        for t in reads:
            self.last_r.setdefault(t, {})[key] = val
        for t in writes:
            self.last_w[t] = (key, val)
            self.last_r[t] = {}

    def op(self, eng, fn, reads=(), writes=(), inc=True):
        waits = self._deps(eng, reads, writes)
        for w in waits:
            self.lists[eng].append(("wait", w[0], w[1]))
        val = self.cnt[eng] + 1
        if inc:
            self.cnt[eng] = val
        self.lists[eng].append(("op", fn, inc))
        self._mark(eng, val, reads, writes)
        self.ninstr += 1

    def dma(self, queue, out, in_, reads=(), writes=(), is_output=False, **kw):
        i = self.dma_rr[queue] % self.n_dma_sems
        self.dma_rr[queue] += 1
        key = ("dma", queue, i)
        prev = self.dma_val.get(key, 0)
        waits = self._deps(queue, reads, writes)
        if prev > 0:
            self._need(queue, key, prev, waits)
        for w in waits:
            self.lists[queue].append(("wait", w[0], w[1]))
        val = prev + 16
        self.dma_val[key] = val
        self.lists[queue].append(("dma", out, in_, key, kw))
        self._mark(key, val, reads, writes)
        if is_output:
            self.out_waits.append((key, val))
        self.ninstr += 1

    def barrier(self):
        keys = [(k, self.cnt[k]) for k in ENGS] + list(self.dma_val.items())
        for e in ENGS:
            waits = []
            for k, v in keys:
                if k != e:
                    self._need(e, k, v, waits)
            for w in waits:
                self.lists[e].append(("wait", w[0], w[1]))

    def tt(self, eng, out, a, b, op, r, w):
        self.op(eng, lambda e: e.tensor_tensor(out=out, in0=a, in1=b, op=op), r, w)

    def ts(self, eng, out, a, s1, s2, op0, op1, r, w):
        if op1 is None:
            self.op(eng, lambda e: e.tensor_scalar(out=out, in0=a, scalar1=s1, scalar2=None, op0=op0), r, w)
        else:
            self.op(eng, lambda e: e.tensor_scalar(out=out, in0=a, scalar1=s1, scalar2=s2, op0=op0, op1=op1), r, w)

    def stt(self, out, a, s, b, op0, op1, r, w):
        self.op("dve", lambda e: e.scalar_tensor_tensor(out=out, in0=a, scalar=s, in1=b, op0=op0, op1=op1), r, w)

    def act(self, out, in_, func, r, w, **kw):
        self.op("act", lambda e: e.activation(out=out, in_=in_, func=func, **kw), r, w)

    def copy(self, eng, out, in_, r, w):
        if eng == "act":
            self.op("act", lambda e: e.activation(out=out, in_=in_, func=AF.Copy), r, w)
        else:
            self.op(eng, lambda e: e.tensor_copy(out=out, in_=in_), r, w)

    def mm(self, out, lhsT, rhs, start, stop, r, w, inc=None):
        if inc is None:
            inc = stop
        self.op("pe", lambda e: e.matmul(out, lhsT, rhs, start=start, stop=stop), r, w, inc=inc)

    def tr(self, out, in_, ident, r, w, inc=True):
        self.op("pe", lambda e: e.transpose(out, in_, ident), r, w, inc=inc)

    def finalize(self):
        nc = self.nc
        keys = list(ENGS) + list(self.dma_val.keys())
        for k in keys:
            nm = "s_" + ("_".join(str(x) for x in k) if isinstance(k, tuple) else k)
            self.sems[k] = self.enter(nc.semaphore(nm))
        for (key, val) in self.out_waits:
            w = []
            self._need("sp", key, val, w)
            for x in w:
                self.lists["sp"].append(("wait", x[0], x[1]))
        block = self.enter(nc.Block())
        engmap = {"pe": block.tensor, "act": block.scalar, "dve": block.vector, "pool": block.gpsimd,
                  "sp": block.sync}

        def make_body(e):
            lst = self.lists[e]
            sem_e = self.sems[e]

            def body(eng):
                for item in lst:
                    if item[0] == "wait":
                        eng.wait_ge(self.sems[item[1]], item[2])
                    elif item[0] == "op":
                        ins = item[1](eng)
                        if item[2]:
                            ins.then_inc(sem_e, 1)
                    else:
                        _, out, in_, key, kw = item
                        eng.dma_start(out=out, in_=in_, **kw).then_inc(self.sems[key], 16)
            return body

        for e in ENGS:
            if self.lists[e]:
                engmap[e](make_body(e))
        self.release(0)
        return nc


def host_consts():
    i = np.arange(128)
    c = {}
    c["iota_f"] = np.broadcast_to(i[None, :].astype(np.float32), (128, 128)).copy()
    c["ident_f"] = np.eye(128, dtype=np.float32)
    c["ident_b"] = np.eye(128).astype(ml_dtypes.bfloat16)
    c["ustrict_b"] = (i[:, None] < i[None, :]).astype(ml_dtypes.bfloat16)
    c["ones_b"] = np.ones((128, 128), dtype=ml_dtypes.bfloat16)
    c["ones_f"] = np.ones((128, 128), dtype=np.float32)
    return c


def load_consts(P, names):
    t = {}
    for n in names:
        dt = BF16 if n.endswith("_b") else F32
        d = P.dram(n, [128, 128], dt, IN)
        s = P.sbuf("c_" + n, [128, 128], dt)
        P.dma("sp", s[:], d[:, :], writes=[n])
        t[n] = s
    return t


def build_mods():
    P = Prog()
    cT_d = P.dram("cT", [128, 8, 4], F32, IN)
    w_d = P.dram("w", [8, 128, 3072], F32, IN)
    b_d = P.dram("b", [4, 3072], F32, IN)
    o_d = P.dram("o", [4, 3072], F32, OUT)
    cT = P.sbuf("cT", [128, 8, 4], F32)
    sc = P.sbuf("sc", [128, 8, 128], F32)
    w = P.sbuf("w", [128, 8, 3072], F32)
    b = P.sbuf("b", [4, 3072], F32)
    o = P.sbuf("o", [4, 3072], F32)
    ps = P.psum("ps", [128, 8, 512], F32)
    P.dma("sp", cT[:], cT_d[:, :, :], writes=["cT"])
    P.dma("sp", b[:], b_d[:, :], writes=["b"])
    for dc in range(8):
        P.dma("sp", w[:, dc, :], w_d[dc], writes=[("w", dc)])
    P.op("dve", lambda e: e.memset(sc[:], 0.0), [], ["sc"])
    P.act(sc[:, :, 0:4], cT[:], AF.Silu, ["cT"], ["sc"])
    for n in range(6):
        for dc in range(8):
            P.mm(ps[:, n, :], sc[:, dc, :], w[:, dc, n * 512:(n + 1) * 512], dc == 0, dc == 7,
                 ["sc", ("w", dc)], [("ps", n)])
        P.tt("dve", o[:, n * 512:(n + 1) * 512], ps[0:4, n, :], b[:, n * 512:(n + 1) * 512], ALU.add,
             [("ps", n), "b"], [("o", n)])
    P.dma("sp", o_d[:, :], o[:], reads=[("o", n) for n in range(6)], is_output=True)
    return P.finalize()


def run_mods(c, ada_w, ada_b):
    nc = build_mods()
    cT = np.ascontiguousarray(c.T.reshape(8, 128, 4).transpose(1, 0, 2))
    in_maps = []
    for i in range(8):
        l, h = i // 2, i % 2
        w = np.ascontiguousarray(ada_w[l][:, h * 3072:(h + 1) * 3072].reshape(8, 128, 3072))
        b = np.ascontiguousarray(np.broadcast_to(ada_b[l][None, h * 3072:(h + 1) * 3072], (4, 3072)))
        in_maps.append({"cT": cT, "w": w, "b": b})
    res = run_bass_kernel_spmd(nc, in_maps, core_ids=list(range(8)))
    mods = np.zeros((4, 4, 6144), np.float32)
    for i in range(8):
        l, h = i // 2, i % 2
        mods[l][:, h * 3072:(h + 1) * 3072] = res.results[i]["o"]
    return mods


NTOK = 2048
NCH = NTOK // 128
TILE_CH = 4
NTILE = NCH // TILE_CH
NEXP = 32
NRING_GU = 12
NBLK = 2
NRING_D = 10


def build_post(n_part, n_exp=NEXP):
    P = Prog()
    x_d = P.dram("x", [NTOK, D], F32, IN)
    y_d = [P.dram("y%d" % i, [NTOK, D], F32, IN) for i in range(n_part)]
    mod_d = P.dram("modbc", [4, D], F32, IN)
    ln_d = P.dram("lnp", [4, D], F32, IN)
    br_d = P.dram("brt", [1, 32], F32, IN)
    wr_d = P.dram("wr", [8, 128, 32], F32, IN)
    wgu_d = P.dram("wgu", [32, 8, 128, 2048], F32, IN)
    bgu_d = P.dram("bgu", [32, 2048], F32, IN)
    wd_d = P.dram("wd", [32, 8, 128, 1024], F32, IN)
    bd_d = P.dram("bd", [32, 1024], F32, IN)
    out_d = P.dram("out", [NTOK, D], F32, OUT)

    C = load_consts(P, ["iota_f", "ident_f", "ident_b", "ustrict_b", "ones_b"])
    acc = P.sbuf("acc", [128, NCH, D], F32)
    hb = P.sbuf("hb", [128, NCH, D], BF16)
    Pm = P.sbuf("Pm", [128, NBLK, NCH, 32], F32)
    Mm = P.sbuf("Mm", [128, NCH, 32], F32)
    Gm = P.sbuf("Gm", [128, NCH, 32], F32)
    gfp = P.sbuf("gfp", [128, D], F32)
    ps = P.psum("ps", [128, 8, 512], F32)

    mk = P.mark()
    xin = [P.sbuf("xin%d" % i, [128, D], F32) for i in range(2)]
    yin = [[P.sbuf("yin%d_%d" % (i, j), [128, D], F32) for j in range(n_part)] for i in range(2)]
    gtp = P.sbuf("gtp", [128, D], F32)
    sfp = P.sbuf("sfp", [128, D], F32)
    shf = P.sbuf("shf", [128, D], F32)
    g1 = P.sbuf("g1", [128, D], F32)
    b1 = P.sbuf("b1", [128, D], F32)
    brt = P.sbuf("brt", [128, 32], F32)
    wr = P.sbuf("wr", [128, 8, 32], F32)
    t1 = P.sbuf("t1", [128, D], F32)
    hf = P.sbuf("hf", [128, D], F32)
    hT = P.sbuf("hT", [128, 8, 128], F32)
    st = P.sbuf("st", [128, 2, 6], F32)
    mv = P.sbuf("mv", [128, 2], F32)
    rstd = P.sbuf("rstd", [128, 1], F32)
    lg = P.sbuf("lg", [128, 32], F32)
    m8 = P.sbuf("m8", [128, 8], F32)
    nmx = P.sbuf("nmx", [128, 1], F32)
    ex = P.sbuf("ex", [128, 32], F32)
    den = P.sbuf("den", [128, 1], F32)
    Mb = P.sbuf("Mb", [128, NCH, 32], BF16)

    P.dma("act", gtp[:], mod_d[0, :].partition_broadcast(128), writes=["gtp"])
    P.dma("act", sfp[:], mod_d[1, :].partition_broadcast(128), writes=["sfp"])
    P.dma("act", shf[:], mod_d[2, :].partition_broadcast(128), writes=["shf"])
    P.dma("act", gfp[:], mod_d[3, :].partition_broadcast(128), writes=["gfp"])
    P.dma("act", g1[:], ln_d[0, :].partition_broadcast(128), writes=["g1"])
    P.dma("act", b1[:], ln_d[1, :].partition_broadcast(128), writes=["b1"])
    P.dma("act", brt[:], br_d[0, :].partition_broadcast(128), writes=["brt"])
    P.dma("act", wr[:], wr_d.rearrange("c p e -> p c e"), writes=["wr"])
    P.ts("pool", gtp[:], gtp[:], 1.0, None, ALU.add, None, ["gtp"], ["gtp"])
    P.ts("pool", sfp[:], sfp[:], 1.0, None, ALU.add, None, ["sfp"], ["sfp"])
    P.ts("pool", gfp[:], gfp[:], 1.0, None, ALU.add, None, ["gfp"], ["gfp"])

    def load_chunk(c):
        b = c % 2
        P.dma("sp", xin[b][:], x_d[c * 128:(c + 1) * 128, :], writes=[("xin", b)])
        for j in range(n_part):
            P.dma("sp", yin[b][j][:], y_d[j][c * 128:(c + 1) * 128, :], writes=[("yin", b, j)])

    load_chunk(0)
    for c in range(NCH):
        b = c % 2
        if c + 1 < NCH:
            load_chunk(c + 1)
        ysum = yin[b][0]
        if n_part == 2:
            P.tt("pool", yin[b][0][:], yin[b][0][:], yin[b][1][:], ALU.add, [("yin", b, 0), ("yin", b, 1)],
                 [("yin", b, 0)])
        P.tt("pool", ysum[:], ysum[:], gtp[:], ALU.mult, [("yin", b, 0), "gtp"], [("yin", b, 0)])
        P.stt(t1[:], xin[b][:], ALPHA, ysum[:], ALU.mult, ALU.add, [("xin", b), ("yin", b, 0)], ["t1"])
        for k in range(2):
            P.op("dve", lambda e, k=k: e.bn_stats(out=st[:, k, :], in_=t1[:, k * 512:(k + 1) * 512]), ["t1"],
                 [("st", k)])
        P.op("dve", lambda e: e.bn_aggr(out=mv[:], in_=st[:].rearrange("p a b -> p (a b)")), [("st", 0), ("st", 1)], ["mv"])
        P.ts("dve", rstd[:], mv[:, 1:2], LN_EPS, None, ALU.add, None, ["mv"], ["rstd"])
        P.act(rstd[:], rstd[:], AF.Sqrt, ["rstd"], ["rstd"])
        P.op("dve", lambda e: e.reciprocal(out=rstd[:], in_=rstd[:]), ["rstd"], ["rstd"])
        P.ts("dve", t1[:], t1[:], mv[:, 0:1], rstd[:, 0:1], ALU.subtract, ALU.mult, ["t1", "mv", "rstd"], ["t1"])
        P.tt("pool", t1[:], t1[:], g1[:], ALU.mult, ["t1", "g1"], ["t1"])
        P.tt("dve", t1[:], t1[:], b1[:], ALU.add, ["t1", "b1"], ["t1"])
        P.op("act", lambda e, c=c: e.mul(acc[:, c, :], t1[:], ALPHA), ["t1"], [("acc", c)])
        P.tt("pool", hf[:], t1[:], sfp[:], ALU.mult, ["t1", "sfp"], ["hf"])
        P.tt("dve", hf[:], hf[:], shf[:], ALU.add, ["hf", "shf"], ["hf"])
        P.copy("act", hb[:, c, :], hf[:], ["hf"], [("hb", c)])
        for dc in range(8):
            P.tr(ps[:, dc // 4, (dc % 4) * 128:(dc % 4 + 1) * 128], hf[:, dc * 128:(dc + 1) * 128], C["ident_f"][:],
                 ["hf", "ident_f"], [("ps", dc // 4)], inc=(dc % 4 == 3))
        P.copy("act", hT[:, 0:4, :], ps[:, 0, :].rearrange("p (a b) -> p a b", a=4), [("ps", 0)], [("hT", 0)])
        P.copy("dve", hT[:, 4:8, :], ps[:, 1, :].rearrange("p (a b) -> p a b", a=4), [("ps", 1)], [("hT", 1)])
        for dc in range(8):
            P.mm(ps[:, 2, 0:32], hT[:, dc, :], wr[:, dc, :], dc == 0, dc == 7, [("hT", dc // 4), "wr"], [("ps", 2)])
        P.tt("dve", lg[:], ps[:, 2, 0:32], brt[:], ALU.add, [("ps", 2), "brt"], ["lg"])
        P.op("dve", lambda e: e.max(out=m8[:], in_=lg[:]), ["lg"], ["m8"])
        P.ts("dve", Mm[:, c, :], lg[:], m8[:, 3:4], None, ALU.is_ge, None, ["lg", "m8"], [("Mm", c)])
        P.ts("dve", nmx[:], m8[:, 0:1], -1.0, None, ALU.mult, None, ["m8"], ["nmx"])
        P.act(ex[:], lg[:], AF.Exp, ["lg", "nmx"], ["ex"], bias=nmx[:, 0:1], scale=1.0)
        P.tt("dve", ex[:], ex[:], Mm[:, c, :], ALU.mult, ["ex", ("Mm", c)], ["ex"])
        P.op("dve", lambda e: e.reduce_sum(out=den[:], in_=ex[:], axis=AX.X), ["ex"], ["den"])
        P.op("dve", lambda e: e.reciprocal(out=den[:], in_=den[:]), ["den"], ["den"])
        P.ts("dve", Gm[:, c, :], ex[:], den[:, 0:1], None, ALU.mult, None, ["ex", "den"], [("Gm", c)])
        P.copy("pool", Mb[:, c, :], Mm[:, c, :], [("Mm", c)], [("Mb", c)])
        if c % TILE_CH == TILE_CH - 1:
            t0 = c - (TILE_CH - 1)
            for cc in range(t0, c + 1):
                n_mm = 1 + (cc - t0)
                P.mm(ps[:, 3, 0:32], C["ustrict_b"][:], Mb[:, cc, :], True, n_mm == 1,
                     ["ustrict_b", ("Mb", cc)], [("ps", 3)])
                for k, c2 in enumerate(range(t0, cc)):
                    P.mm(ps[:, 3, 0:32], C["ones_b"][:], Mb[:, c2, :], False, k == n_mm - 2,
                         ["ones_b", ("Mb", c2)], [("ps", 3)])
                for blk in range(NBLK):
                    P.ts("dve", Pm[:, blk, cc, :], ps[:, 3, 0:32], -128.0 * blk, None, ALU.add, None, [("ps", 3)], [("Pm", cc)])
    P.barrier()
    P.release(mk)

    mk = P.mark()
    wgu = P.sbuf("wgu", [128, NRING_GU, 2048], BF16)
    wd = P.sbuf("wd", [128, NRING_D, 1024], BF16)
    bgu = P.sbuf("bgu", [128, 2048], F32)
    bd = P.sbuf("bd", [128, 1024], F32)
    sel = P.sbuf("sel", [128, TILE_CH, 128], BF16)
    selg = P.sbuf("selg", [128, TILE_CH, 128], BF16)
    selgT = P.sbuf("selgT", [128, TILE_CH, 128], BF16)
    xgT = P.sbuf("xgT", [128, 8, 128], BF16)
    gg = P.sbuf("gg", [128, 512], F32)
    uu = P.sbuf("uu", [128, 512], F32)
    sg = P.sbuf("sg", [128, 512], F32)
    actb = P.sbuf("actb", [128, 1024], BF16)
    actT = P.sbuf("actT", [128, 8, 128], BF16)
    yb = P.sbuf("yb", [128, 1024], BF16)
    psb = ps[:, 0, :].bitcast(BF16)

    def load_expert(e):
        for dc in range(8):
            s = (e * 8 + dc) % NRING_GU
            P.dma("pool", wgu[:, s, :], wgu_d[e, dc], writes=[("wgu", s)])
        for fc in range(8):
            s = (e * 8 + fc) % NRING_D
            P.dma("pool", wd[:, s, :], wd_d[e, fc], writes=[("wd", s)])

    for e in range(n_exp):
        load_expert(e)
        P.dma("sp", bgu[:], bgu_d[e, :].partition_broadcast(128), writes=["bgu"])
        P.dma("sp", bd[:], bd_d[e, :].partition_broadcast(128), writes=["bd"])
        P.tt("dve", bd[:], bd[:], gfp[:], ALU.mult, ["bd", "gfp"], ["bd"])
        for T in range(NTILE):
            c0 = T * TILE_CH
            for blk in range(NBLK):
                for k in range(TILE_CH):
                    c = c0 + k
                    P.ts("dve", sel[:, k, :], C["iota_f"][:], Pm[:, blk, c, e:e + 1], Mm[:, c, e:e + 1], ALU.is_equal, ALU.mult,
                         ["iota_f", ("Pm", c), ("Mm", c)], [("sel", k)])
                    P.ts("dve", selg[:, k, :], C["iota_f"][:], Pm[:, blk, c, e:e + 1], Gm[:, c, e:e + 1], ALU.is_equal, ALU.mult,
                         ["iota_f", ("Pm", c), ("Gm", c)], [("selg", k)])
                for k in range(TILE_CH):
                    P.tr(psb[:, k * 128:(k + 1) * 128], selg[:, k, :], C["ident_b"][:], [("selg", k), "ident_b"], [("ps", 0)],
                         inc=(k == TILE_CH - 1))
                P.copy("act", selgT[:].rearrange("p a b -> p (a b)"), psb[:, 0:512], [("ps", 0)], ["selgT"])
                for dc in range(8):
                    for k in range(TILE_CH):
                        P.mm(ps[:, 1 + dc // 4, (dc % 4) * 128:(dc % 4 + 1) * 128], hb[:, c0 + k, dc * 128:(dc + 1) * 128],
                             sel[:, k, :], k == 0, k == TILE_CH - 1, [("hb", c0 + k), ("sel", k)], [("ps", 1 + dc // 4)],
                             inc=(k == TILE_CH - 1 and dc % 4 == 3))
                P.copy("act", xgT[:, 0:4, :], ps[:, 1, :].rearrange("p (a b) -> p a b", a=4), [("ps", 1)], [("xgT", 0)])
                P.copy("dve", xgT[:, 4:8, :], ps[:, 2, :].rearrange("p (a b) -> p a b", a=4), [("ps", 2)], [("xgT", 1)])
                for h in range(2):
                    for part in range(2):
                        col = part * 1024 + h * 512
                        for dc in range(8):
                            s = (e * 8 + dc) % NRING_GU
                            P.mm(ps[:, 3 + part, :], xgT[:, dc, :], wgu[:, s, col:col + 512], dc == 0, dc == 7,
                                 [("xgT", dc // 4), ("wgu", s)], [("ps", 3 + part)])
                    P.tt("dve", gg[:], ps[:, 3, :], bgu[:, h * 512:(h + 1) * 512], ALU.add, [("ps", 3), "bgu"], ["gg"])
                    P.tt("dve", uu[:], ps[:, 4, :], bgu[:, 1024 + h * 512:1024 + (h + 1) * 512], ALU.add,
                         [("ps", 4), "bgu"], ["uu"])
                    P.ts("dve", gg[:], gg[:], 7.0, None, ALU.min, None, ["gg"], ["gg"])
                    P.ts("dve", uu[:], uu[:], -7.0, 7.0, ALU.max, ALU.min, ["uu"], ["uu"])
                    P.act(sg[:], gg[:], AF.Sigmoid, ["gg"], ["sg"], scale=1.702)
                    P.tt("dve", sg[:], sg[:], gg[:], ALU.mult, ["sg", "gg"], ["sg"])
                    P.stt(actb[:, h * 512:(h + 1) * 512], uu[:], 1.0, sg[:], ALU.add, ALU.mult, ["uu", "sg"], [("actb", h)])
                for fc in range(8):
                    P.tr(psb[:, fc * 128:(fc + 1) * 128], actb[:, fc * 128:(fc + 1) * 128], C["ident_b"][:],
                         [("actb", fc // 4), "ident_b"], [("ps", 0)], inc=(fc == 7))
                P.copy("act", actT[:].rearrange("p a b -> p (a b)"), psb[:, :], [("ps", 0)], ["actT"])
                for dh in range(2):
                    for fc in range(8):
                        s = (e * 8 + fc) % NRING_D
                        P.mm(ps[:, 5 + dh, :], actT[:, fc, :], wd[:, s, dh * 512:(dh + 1) * 512], fc == 0, fc == 7,
                             ["actT", ("wd", s)], [("ps", 5 + dh)])
                for dh in range(2):
                    P.tt("dve", gg[:], ps[:, 5 + dh, :], gfp[:, dh * 512:(dh + 1) * 512], ALU.mult, [("ps", 5 + dh), "gfp"], ["gg"])
                    P.tt("dve", yb[:, dh * 512:(dh + 1) * 512], gg[:], bd[:, dh * 512:(dh + 1) * 512], ALU.add, ["gg", "bd"], ["yb"])
                for k in range(TILE_CH):
                    c = c0 + k
                    for dh in range(2):
                        bank = 7 if (k * 2 + dh) % 2 == 0 else 1
                        P.mm(ps[:, bank, :], selgT[:, k, :], yb[:, dh * 512:(dh + 1) * 512], True, True,
                             ["selgT", "yb"], [("ps", bank)])
                        P.tt("dve", acc[:, c, dh * 512:(dh + 1) * 512], acc[:, c, dh * 512:(dh + 1) * 512], ps[:, bank, :],
                             ALU.add, [("acc", c), ("ps", bank)], [("acc", c)])
    P.barrier()
    P.release(mk)

    g2 = P.sbuf("g2", [128, D], F32)
    b2 = P.sbuf("b2", [128, D], F32)
    ob = [P.sbuf("ob%d" % i, [128, D], F32) for i in range(2)]
    st2 = P.sbuf("st2", [128, 2, 6], F32)
    mv2 = P.sbuf("mv2", [128, 2], F32)
    rs2 = P.sbuf("rs2", [128, 1], F32)
    P.dma("act", g2[:], ln_d[2, :].partition_broadcast(128), writes=["g2"])
    P.dma("act", b2[:], ln_d[3, :].partition_broadcast(128), writes=["b2"])
    for c in range(NCH):
        b = c % 2
        for k in range(2):
            P.op("dve", lambda e, k=k, c=c: e.bn_stats(out=st2[:, k, :], in_=acc[:, c, k * 512:(k + 1) * 512]),
                 [("acc", c)], [("st2", k)])
        P.op("dve", lambda e: e.bn_aggr(out=mv2[:], in_=st2[:].rearrange("p a b -> p (a b)")), [("st2", 0), ("st2", 1)], ["mv2"])
        P.ts("dve", rs2[:], mv2[:, 1:2], LN_EPS, None, ALU.add, None, ["mv2"], ["rs2"])
        P.act(rs2[:], rs2[:], AF.Sqrt, ["rs2"], ["rs2"])
        P.op("dve", lambda e: e.reciprocal(out=rs2[:], in_=rs2[:]), ["rs2"], ["rs2"])
        P.ts("dve", ob[b][:], acc[:, c, :], mv2[:, 0:1], rs2[:, 0:1], ALU.subtract, ALU.mult, [("acc", c), "mv2", "rs2"],
             [("ob", b)])
        P.tt("pool", ob[b][:], ob[b][:], g2[:], ALU.mult, [("ob", b), "g2"], [("ob", b)])
        P.tt("dve", ob[b][:], ob[b][:], b2[:], ALU.add, [("ob", b), "b2"], [("ob", b)])
        P.dma("sp", out_d[c * 128:(c + 1) * 128, :], ob[b][:], reads=[("ob", b)], is_output=True)
    return P.finalize()


def host_consts_post():
    c = host_consts()
    return {k: c[k] for k in ["iota_f", "ident_f", "ident_b", "ustrict_b", "ones_b"]}


def post_inputs(x, ys, md, ln_g, ln_b, wr, br, wgu, bgu, wd, bd):
    im = {"x": np.ascontiguousarray(x)}
    for i, y in enumerate(ys):
        im["y%d" % i] = np.ascontiguousarray(y)
    im["modbc"] = np.ascontiguousarray(np.stack([md[2], md[4], md[3], md[5]]))
    im["lnp"] = np.ascontiguousarray(np.stack([ln_g[0], ln_b[0], ln_g[1], ln_b[1]]))
    im["brt"] = np.ascontiguousarray(br.reshape(1, 32))
    im["wr"] = np.ascontiguousarray(wr.reshape(8, 128, 32))
    im["wgu"] = np.ascontiguousarray(wgu.reshape(32, 8, 128, 2048))
    im["bgu"] = np.ascontiguousarray(bgu)
    im["wd"] = np.ascontiguousarray(wd.reshape(32, 8, 128, 1024))
    im["bd"] = np.ascontiguousarray(bd)
    return im


SEQ = 4096
NTB = SEQ // 512
MLA_SCALE = 192.0 ** -0.5
RMS_EPS = 1e-6


def build_mla():
    P = Prog()
    xT_d = P.dram("xT", [8, 128, SEQ], F32, IN)
    modc_d = P.dram("modc", [128, 8, 2], F32, IN)
    wdq_d = P.dram("wdq", [8, 128, 704], F32, IN)
    gq_d = P.dram("gq", [128, 3], F32, IN)
    gkv_d = P.dram("gkv", [128, 2], F32, IN)
    wuq_d = P.dram("wuq", [3, 128, 768], F32, IN)
    wukv_d = P.dram("wukv", [2, 128, 1024], F32, IN)
    wo_d = P.dram("wo", [4, 128, 1024], F32, IN)
    cs_d = P.dram("cs", [2, 64, SEQ], F32, IN)
    mask_d = P.dram("masks", [4, 128, 512], BF16, IN)
    y_d = P.dram("y", [SEQ, D], F32, OUT)
    C = load_consts(P, ["ones_b", "ones_f"])
    ps = P.psum("ps", [128, 8, 512], F32)

    modc = P.sbuf("modc", [128, 8, 2], F32)
    gq = P.sbuf("gq", [128, 3], F32)
    gkv = P.sbuf("gkv", [128, 2], F32)
    wuq = P.sbuf("wuq", [128, 3, 768], BF16)
    wusw = P.sbuf("wusw", [128, 3, 4, 64], BF16)
    wukv = P.sbuf("wukv", [128, 2, 1024], BF16)
    wo = P.sbuf("wo", [128, 4, 1024], BF16)
    C2 = P.sbuf("C2", [64, SEQ], F32)
    S2 = P.sbuf("S2", [64, SEQ], F32)
    masks = P.sbuf("masks", [128, 4, 512], BF16)
    cqn = P.sbuf("cqn", [128, 3, SEQ], BF16)
    ckvn = P.sbuf("ckvn", [128, 2, SEQ], BF16)
    kr = P.sbuf("kr", [65, SEQ], BF16)
    krsq = P.sbuf("krsq", [64, SEQ], BF16)

    P.dma("sp", modc[:], modc_d[:, :, :], writes=["modc"])
    P.dma("sp", gq[:], gq_d[:, :], writes=["gq"])
    P.dma("sp", gkv[:], gkv_d[:, :], writes=["gkv"])
    P.dma("pool", wuq[:], wuq_d.rearrange("c p f -> p c f"), writes=["wuq"])
    P.dma("pool", wukv[:], wukv_d.rearrange("c p f -> p c f"), writes=["wukv"])
    P.dma("pool", wo[:], wo_d.rearrange("c p f -> p c f"), writes=["wo"])
    P.dma("act", C2[:], cs_d[0], writes=["C2"])
    P.dma("act", S2[:], cs_d[1], writes=["S2"])
    P.dma("act", masks[:], mask_d.rearrange("c p f -> p c f"), writes=["masks"])
    P.ts("dve", modc[:, :, 0:1], modc[:, :, 0:1], 1.0, None, ALU.add, None, ["modc"], ["modc"])
    for h in range(4):
        P.ts("dve", wusw[:, :, h, 0:32], wuq[:, :, h * 192 + 160:h * 192 + 192], -1.0, None, ALU.mult, None, ["wuq"], ["wusw"])
        P.copy("dve", wusw[:, :, h, 32:64], wuq[:, :, h * 192 + 128:h * 192 + 160], ["wuq"], ["wusw"])
    P.op("dve", lambda e: e.memset(kr[64:65, :], 1.0), [], ["kr64"])

    mk = P.mark()
    wdq = P.sbuf("wdq", [128, 8, 704], BF16)
    wdsw = P.sbuf("wdsw", [128, 8, 64], BF16)
    P.dma("pool", wdq[:], wdq_d.rearrange("c p f -> p c f"), writes=["wdq"])
    P.ts("dve", wdsw[:, :, 0:32], wdq[:, :, 672:704], -1.0, None, ALU.mult, None, ["wdq"], ["wdsw"])
    P.copy("dve", wdsw[:, :, 32:64], wdq[:, :, 640:672], ["wdq"], ["wdsw"])
    xin = [P.sbuf("xin%d" % i, [128, 512], F32) for i in range(4)]
    hT = [P.sbuf("hT%d" % i, [128, 8, 512], BF16) for i in range(2)]
    cf = P.sbuf("cf", [128, 5, 512], F32)
    sq = P.sbuf("sq", [128, 5, 512], F32)
    krf = P.sbuf("krf", [64, 2, 512], F32)
    rs = P.sbuf("rs", [128, 2, 512], F32)
    tmp = P.sbuf("tmp", [64, 512], F32)

    def load_x(i):
        tb_, dc_ = i // 8, i % 8
        if tb_ < NTB:
            P.dma("sp", xin[i % 4][:], xT_d[dc_][:, tb_ * 512:(tb_ + 1) * 512], writes=[("xin", i % 4)])

    for i in range(4):
        load_x(i)
    for tb in range(NTB):
        bb = tb % 2
        tsl = slice(tb * 512, (tb + 1) * 512)
        for dc in range(8):
            sl = (tb * 8 + dc) % 4
            P.ts("dve" if dc % 2 == 0 else "pool", hT[bb][:, dc, :], xin[sl][:], modc[:, dc, 0:1], modc[:, dc, 1:2],
                 ALU.mult, ALU.add, [("xin", sl), "modc"], [("hT", bb, dc)])
            load_x(tb * 8 + dc + 4)
        for f in range(5):
            bank = f % 4
            for dc in range(8):
                P.mm(ps[:, bank, :], wdq[:, dc, f * 128:(f + 1) * 128], hT[bb][:, dc, :], dc == 0, dc == 7,
                     ["wdq", ("hT", bb, dc)], [("ps", bank)])
            P.copy("act", cf[:, f, :], ps[:, bank, :], [("ps", bank)], [("cf", f)])
            P.act(sq[:, f, :], cf[:, f, :], AF.Square, [("cf", f)], [("sq", f)])
        for v in range(2):
            bank = 4 + v
            for dc in range(8):
                lw = wdq[:, dc, 640:704] if v == 0 else wdsw[:, dc, :]
                P.mm(ps[0:64, bank, :], lw, hT[bb][:, dc, :], dc == 0, dc == 7, ["wdq", "wdsw", ("hT", bb, dc)], [("ps", bank)])
            P.copy("act", krf[:, v, :], ps[0:64, bank, :], [("ps", bank)], [("krf", v)])
        for n, (f0, nf, gt, dst) in enumerate([(0, 3, gq, cqn), (3, 2, gkv, ckvn)]):
            bank = 6 + n
            for j in range(nf):
                P.mm(ps[:, bank, :], C["ones_f"][:], sq[:, f0 + j, :], j == 0, j == nf - 1, ["ones_f", ("sq", f0 + j)], [("ps", bank)])
            P.ts("dve", rs[:, n, :], ps[:, bank, :], 1.0 / (nf * 128), RMS_EPS, ALU.mult, ALU.add, [("ps", bank)], [("rs", n)])
            P.act(rs[:, n, :], rs[:, n, :], AF.Sqrt, [("rs", n)], [("rs", n)])
            P.op("dve", lambda e, n=n: e.reciprocal(out=rs[:, n, :], in_=rs[:, n, :]), [("rs", n)], [("rs", n)])
            for j in range(nf):
                P.stt(dst[:, j, tsl], cf[:, f0 + j, :], gt[:, j:j + 1], rs[:, n, :], ALU.mult, ALU.mult,
                      [("cf", f0 + j), ("rs", n), "gq", "gkv"], [("cn", n, j, tb)])
        P.tt("pool", krf[:, 0, :], krf[:, 0, :], C2[:, tsl], ALU.mult, [("krf", 0), "C2"], [("krf", 0)])
        P.tt("pool", krf[:, 1, :], krf[:, 1, :], S2[:, tsl], ALU.mult, [("krf", 1), "S2"], [("krf", 1)])
        P.tt("dve", tmp[:], krf[:, 0, :], krf[:, 1, :], ALU.add, [("krf", 0), ("krf", 1)], ["tmp"])
        P.copy("pool", kr[0:64, tsl], tmp[:], ["tmp"], [("kr", tb)])
        P.act(krsq[:, tsl], tmp[:], AF.Square, ["tmp"], [("krsq", tb)])
    P.barrier()
    P.release(mk)

    oT = P.sbuf("oT", [128, 4, SEQ], BF16)
    qn = P.sbuf("qn", [128, SEQ], BF16)
    qr = P.sbuf("qr", [65, SEQ], BF16)
    kn = P.sbuf("kn", [128, SEQ], BF16)
    vt = P.sbuf("vt", [128, SEQ // 128, 128], BF16)
    qsq = P.sbuf("qsq", [128, 2, 512], F32)
    ksq = P.sbuf("ksq", [128, 512], F32)
    qrf = P.sbuf("qrf", [64, 2, 512], F32)
    qtmp = P.sbuf("qtmp", [64, 512], F32)
    nq2 = P.sbuf("nq2", [65, SEQ], BF16)
    kmx = P.sbuf("kmx", [128, 1], F32)
    kmt = P.sbuf("kmt", [128, 1], F32)
    pT = [P.sbuf("pT%d" % i, [128, 512], BF16) for i in range(3)]
    rD = P.sbuf("rD", [128, 512], F32)
    ycp = [P.sbuf("ycp%d" % i, [128, 512], F32) for i in range(2)]
    npt = 0
    for h in range(4):
        P.op("dve", lambda e: e.memset(kmx[:], 0.0), [], ["kmx"])
        for tb in range(NTB):
            tsl = slice(tb * 512, (tb + 1) * 512)
            for j in range(3):
                P.mm(ps[:, 0, :], wuq[:, j, h * 192:h * 192 + 128], cqn[:, j, tsl], j == 0, j == 2,
                     ["wuq", ("cn", 0, j, tb)], [("ps", 0)])
            P.copy("act", qn[:, tsl], ps[:, 0, :], [("ps", 0)], [("qn", tb)])
            P.act(qsq[:, 0, :], ps[:, 0, :], AF.Square, [("ps", 0)], [("qsq", 0)])
            for v in range(2):
                for j in range(3):
                    lw = wuq[:, j, h * 192 + 128:h * 192 + 192] if v == 0 else wusw[:, j, h, :]
                    P.mm(ps[0:64, 1 + v, :], lw, cqn[:, j, tsl], j == 0, j == 2, ["wuq", "wusw", ("cn", 0, j, tb)], [("ps", 1 + v)])
            P.tt("dve", qrf[:, 0, :], ps[0:64, 1, :], C2[:, tsl], ALU.mult, [("ps", 1), "C2"], [("qrf", 0)])
            P.tt("dve", qrf[:, 1, :], ps[0:64, 2, :], S2[:, tsl], ALU.mult, [("ps", 2), "S2"], [("qrf", 1)])
            P.tt("pool", qtmp[:], qrf[:, 0, :], qrf[:, 1, :], ALU.add, [("qrf", 0), ("qrf", 1)], ["qtmp"])
            P.copy("pool", qr[0:64, tsl], qtmp[:], ["qtmp"], [("qr", tb)])
            P.act(qsq[0:64, 1, :], qtmp[:], AF.Square, ["qtmp"], [("qsq", 1)])
            P.mm(ps[:, 3, :], C["ones_f"][:], qsq[:, 0, :], True, False, ["ones_f", ("qsq", 0)], [("ps", 3)])
            P.mm(ps[:, 3, :], C["ones_f"][0:64, :], qsq[0:64, 1, :], False, True, ["ones_f", ("qsq", 1)], [("ps", 3)])
            P.copy("act", nq2[64:65, tsl], ps[64:65, 3, :], [("ps", 3)], [("nq2", tb)])
            for j in range(2):
                P.mm(ps[:, 4, :], wukv[:, j, h * 256:h * 256 + 128], ckvn[:, j, tsl], j == 0, j == 1,
                     ["wukv", ("cn", 1, j, tb)], [("ps", 4)])
            P.copy("act", kn[:, tsl], ps[:, 4, :], [("ps", 4)], [("kn", tb)])
            P.act(ksq[:], ps[:, 4, :], AF.Square, [("ps", 4)], ["ksq"])
            P.mm(ps[:, 5, :], C["ones_f"][:], ksq[:], True, False, ["ones_f", "ksq"], [("ps", 5)])
            P.mm(ps[:, 5, :], C["ones_b"][0:64, :], krsq[:, tsl], False, True, ["ones_b", ("krsq", tb)], [("ps", 5)])
            P.op("dve", lambda e: e.reduce_max(out=kmt[:], in_=ps[:, 5, :], axis=AX.X), [("ps", 5)], ["kmt"])
            P.tt("dve", kmx[:], kmx[:], kmt[:], ALU.max, ["kmx", "kmt"], ["kmx"])
            for tq in range(4):
                tc_ = tb * 4 + tq
                for j in range(2):
                    P.mm(ps[:, 6, tq * 128:(tq + 1) * 128], ckvn[:, j, tc_ * 128:(tc_ + 1) * 128],
                         wukv[:, j, h * 256 + 128:h * 256 + 256], j == 0, j == 1, ["wukv", ("cn", 1, j, tb)], [("ps", 6)],
                         inc=(j == 1 and tq == 3))
            P.copy("dve", vt[:, tb * 4:(tb + 1) * 4, :], ps[:, 6, :].rearrange("p (a b) -> p a b", a=4), [("ps", 6)], [("vt", tb)])
        for tb in range(NTB):
            tsl = slice(tb * 512, (tb + 1) * 512)
            P.ts("dve", nq2[64:65, tsl], nq2[64:65, tsl], kmx[64:65, 0:1], None, ALU.mult, None, [("nq2", tb), "kmx"], [("nq2", tb)])
            P.act(nq2[64:65, tsl], nq2[64:65, tsl], AF.Sqrt, [("nq2", tb)], [("nq2", tb)])
            P.ts("dve", qr[64:65, tsl], nq2[64:65, tsl], -1.0, None, ALU.mult, None, [("nq2", tb)], [("qr64", tb)])
        for qg in range(NTB):
            qsl = slice(qg * 512, (qg + 1) * 512)
            nkc = 4 * qg + 4
            for kc in range(nkc):
                ksl = slice(kc * 128, (kc + 1) * 128)
                sb = kc % 2
                P.mm(ps[:, sb, :], kn[:, ksl], qn[:, qsl], True, False, [("kn", kc // 4), ("qn", qg)], [("ps", sb)])
                P.mm(ps[:, sb, :], kr[0:65, ksl], qr[0:65, qsl], False, True, [("kr", kc // 4), "kr64", ("qr", qg), ("qr64", qg)], [("ps", sb)])
                pt = pT[npt % 3]
                ptk = ("pT", npt % 3)
                npt += 1
                P.act(pt[:], ps[:, sb, :], AF.Exp, [("ps", sb)], [ptk], scale=MLA_SCALE)
                if kc >= 4 * qg:
                    P.tt("dve", pt[:], pt[:], masks[:, kc - 4 * qg, :], ALU.mult, [ptk, "masks"], [ptk])
                P.mm(ps[:, 2, :], vt[:, kc, :], pt[:], kc == 0, kc == nkc - 1, [("vt", kc // 4), ptk], [("ps", 2)], inc=False)
                P.mm(ps[:, 3, :], C["ones_b"][:], pt[:], kc == 0, kc == nkc - 1, ["ones_b", ptk], [("ps", 3)], inc=True)
            P.op("dve", lambda e: e.reciprocal(out=rD[:], in_=ps[:, 3, :]), [("ps", 3)], ["rD"])
            P.tt("dve", oT[:, h, qsl], ps[:, 2, :], rD[:], ALU.mult, [("ps", 2), "rD"], [("oT", h, qg)])
    for tcn in range(SEQ // 128):
        bb = tcn % 2
        for dh in range(2):
            for h in range(4):
                P.mm(ps[:, 4 + dh, :], oT[:, h, tcn * 128:(tcn + 1) * 128], wo[:, h, dh * 512:(dh + 1) * 512], h == 0, h == 3,
                     [("oT", h, tcn // 4), "wo"], [("ps", 4 + dh)])
        P.copy("act", ycp[0][:], ps[:, 4, :], [("ps", 4)], [("ycp", 0)])
        P.copy("dve", ycp[1][:], ps[:, 5, :], [("ps", 5)], [("ycp", 1)])
        P.dma("sp", y_d[tcn * 128:(tcn + 1) * 128, 0:512], ycp[0][:], reads=[("ycp", 0)], is_output=True)
        P.dma("sp", y_d[tcn * 128:(tcn + 1) * 128, 512:1024], ycp[1][:], reads=[("ycp", 1)], is_output=True)
    return P.finalize()


def rope_tables_np():
    pos = np.arange(SEQ, dtype=np.float32)
    inv = (np.float32(10000.0) ** (-np.arange(0, 64, 2, dtype=np.float32) / np.float32(64))).astype(np.float32)
    ang = pos[:, None] * inv[None, :]
    cos, sin = np.cos(ang).astype(np.float32), np.sin(ang).astype(np.float32)
    return np.ascontiguousarray(np.stack([np.concatenate([cos.T, cos.T], 0), np.concatenate([sin.T, sin.T], 0)]))


def mla_masks_np():
    k = np.arange(128)[:, None]
    q = np.arange(512)[None, :]
    m = np.stack([((128 * i + k) // 64 <= q // 64) for i in range(4)]).astype(ml_dtypes.bfloat16)
    return np.ascontiguousarray(m)


def pcol(v, n):
    return np.ascontiguousarray(v.reshape(n, 128).T)


def mla_inputs(xb, md, w_dqkv, g_q, g_kv, w_uq, w_ukv, w_o, g):
    hs = slice(4 * g, 4 * g + 4)
    im = {}
    im["xT"] = np.ascontiguousarray(xb.T.reshape(8, 128, SEQ))
    im["modc"] = np.ascontiguousarray(np.stack([pcol(md[1], 8), pcol(md[0], 8)], axis=-1))
    im["wdq"] = np.ascontiguousarray(w_dqkv.reshape(8, 128, 704))
    im["gq"] = pcol(g_q, 3)
    im["gkv"] = pcol(g_kv, 2)
    im["wuq"] = np.ascontiguousarray(w_uq.reshape(384, 8, 192)[:, hs, :].reshape(3, 128, 768))
    im["wukv"] = np.ascontiguousarray(w_ukv.reshape(256, 8, 256)[:, hs, :].reshape(2, 128, 1024))
    im["wo"] = np.ascontiguousarray(w_o.reshape(8, 128, 1024)[hs])
    im["cs"] = rope_tables_np()
    im["masks"] = mla_masks_np()
    c = host_consts()
    im["ones_b"] = c["ones_b"]
    im["ones_f"] = c["ones_f"]
    return im


def build_pool():
    P = Prog()
    NT = 2048
    W = NT + 16
    xT_d = P.dram("xT", [8, 128, W], F32, IN)
    modc_d = P.dram("modc", [128, 8, 2], F32, IN)
    hv_d = P.dram("hv", [128, 1], F32, IN)
    corr_d = P.dram("corr", [128, 8, 16], F32, IN)
    wp_d = P.dram("wp", [4, 2, 128, 256], F32, IN)
    csc_d = P.dram("csc", [128, 8], F32, IN)
    yT_d = P.dram("yT", [8, 128, NT], F32, OUT)
    ps = P.psum("ps", [128, 8, 512], F32)
    modc = P.sbuf("modc", [128, 8, 2], F32)
    hv = P.sbuf("hv", [128, 1], F32)
    corr = P.sbuf("corr", [128, 8, 16], F32)
    wp = P.sbuf("wp", [128, 4, 2, 256], BF16)
    csc = P.sbuf("csc", [128, 8], F32)
    xin = [P.sbuf("xin%d" % i, [128, W], F32) for i in range(2)]
    hh = [P.sbuf("hh%d" % i, [128, W], F32) for i in range(2)]
    sa = P.sbuf("sa", [128, W], F32)
    sb_ = P.sbuf("sbb", [128, W], F32)
    dT = P.sbuf("dT", [128, 8, NT], BF16)
    yo = [P.sbuf("yo%d" % i, [128, NT], F32) for i in range(2)]
    P.dma("sp", modc[:], modc_d[:, :, :], writes=["modc"])
    P.dma("sp", hv[:], hv_d[:, :], writes=["hv"])
    P.dma("sp", corr[:], corr_d[:, :, :], writes=["corr"])
    P.dma("sp", csc[:], csc_d[:, :], writes=["csc"])
    P.dma("pool", wp[:], wp_d.rearrange("g c p f -> p g c f"), writes=["wp"])
    P.ts("dve", modc[:, :, 0:1], modc[:, :, 0:1], 1.0, None, ALU.add, None, ["modc"], ["modc"])
    P.dma("sp", xin[0][:], xT_d[0], writes=[("xin", 0)])
    for dc in range(8):
        b = dc % 2
        if dc + 1 < 8:
            P.dma("sp", xin[1 - b][:], xT_d[dc + 1], writes=[("xin", 1 - b)])
        gi = dc // 2
        win = 2 << gi
        h = hh[b]
        P.ts("dve", h[:], xin[b][:], modc[:, dc, 0:1], modc[:, dc, 1:2], ALU.mult, ALU.add, [("xin", b), "modc"], [("hh", b)])
        P.ts("dve", h[:, 0:16], h[:, 0:16], hv[:, 0:1], None, ALU.mult, None, [("hh", b), "hv"], [("hh", b)])
        cur, curk = h, ("hh", b)
        sh = 1
        nxt = [(sa, "sa"), (sb_, "sbb")]
        k = 0
        while sh < win:
            dst, dk = nxt[k % 2]
            P.tt("pool" if k % 2 == 0 else "dve", dst[:, sh:W], cur[:, sh:W], cur[:, 0:W - sh], ALU.add, [curk], [dk])
            cur, curk = dst, dk
            sh *= 2
            k += 1
        dst, dk = nxt[k % 2]
        P.ts("dve", dst[:, 16:W], cur[:, 16:W], 1.0 / win, None, ALU.mult, None, [curk], [dk])
        P.tt("dve", dst[:, 16:32], dst[:, 16:32], corr[:, dc, :], ALU.mult, [dk, "corr"], [dk])
        P.tt("dve", dT[:, dc, :], dst[:, 16:W], h[:, 16:W], ALU.subtract, [dk, ("hh", b)], [("dT", dc)])
    for oc in range(8):
        gi = oc // 2
        ob = oc % 2
        for tb in range(4):
            bank = (oc * 4 + tb) % 8
            for cc in range(2):
                P.mm(ps[:, bank, :], wp[:, gi, cc, (oc % 2) * 128:(oc % 2 + 1) * 128], dT[:, gi * 2 + cc, tb * 512:(tb + 1) * 512],
                     cc == 0, cc == 1, ["wp", ("dT", gi * 2 + cc)], [("ps", bank)])
            P.ts("dve" if tb % 2 == 0 else "act_", yo[ob][:, tb * 512:(tb + 1) * 512], ps[:, bank, :], csc[:, oc:oc + 1], None,
                 ALU.mult, None, [("ps", bank), "csc"], [("yo", ob)]) if tb % 2 == 0 else \
                P.act(yo[ob][:, tb * 512:(tb + 1) * 512], ps[:, bank, :], AF.Copy, [("ps", bank), "csc"], [("yo", ob)], scale=csc[:, oc:oc + 1])
        P.dma("sp", yT_d[oc], yo[ob][:], reads=[("yo", ob)], is_output=True)
    return P.finalize()


def pool_inputs(xb, md, w_pool, ch_scale, g):
    NT = 2048
    im = {}
    xT = xb.T
    if g == 0:
        xh = np.concatenate([np.zeros((1024, 16), np.float32), xT[:, 0:NT]], axis=1)
    else:
        xh = xT[:, g * NT - 16:(g + 1) * NT]
    im["xT"] = np.ascontiguousarray(xh.reshape(8, 128, NT + 16))
    im["modc"] = np.ascontiguousarray(np.stack([pcol(md[1], 8), pcol(md[0], 8)], axis=-1))
    im["hv"] = np.full((128, 1), 0.0 if g == 0 else 1.0, np.float32)
    corr = np.ones((128, 8, 16), np.float32)
    if g == 0:
        t = np.arange(16)
        for dc in range(8):
            win = 2 << (dc // 2)
            corr[:, dc, :] = (win / np.minimum(t + 1, win)).astype(np.float32)[None, :]
    im["corr"] = corr
    im["wp"] = np.ascontiguousarray(w_pool.reshape(4, 2, 128, 256))
    im["csc"] = pcol(ch_scale, 8)
    return im


GDN_NSET = 3


def build_gdn(stage=99):
    P = Prog()
    xT_d = P.dram("xT", [8, 128, SEQ], F32, IN)
    modc_d = P.dram("modc", [128, 8, 2], F32, IN)
    win_d = P.dram("win", [8, 128, 3088], F32, IN)
    wcv_d = P.dram("wcv", [128, 16, 4], F32, IN)
    hp_d = P.dram("hp", [8, 2], F32, IN)
    gn_d = P.dram("gn", [1, 128], F32, IN)
    wo_d = P.dram("wo", [8, 128, 1024], F32, IN)
    oh_d = P.dram("oh", [8, 8, 128], F32, IN)
    mk_d = P.dram("gmask", [3, 64, 64], F32, IN)
    sm_d = P.dram("smask", [8, 512], F32, IN)
    y_d = P.dram("y", [SEQ, D], F32, OUT)
    C = load_consts(P, ["ident_f", "ident_b", "ones_f"])
    ps = P.psum("ps", [128, 8, 512], F32)
    psb5 = ps[:, 5, :].bitcast(BF16)

    modc = P.sbuf("modc", [128, 8, 2], F32)
    win = P.sbuf("win", [128, 8, 3088], BF16)
    wcv = P.sbuf("wcv", [128, 16, 4], F32)
    hp = P.sbuf("hp", [8, 2], F32)
    nea = P.sbuf("nea", [8, 1], F32)
    gnb = P.sbuf("gnb", [64, 128], F32)
    wo = P.sbuf("wo", [128, 8, 1024], BF16)
    OH = P.sbuf("OH", [8, 8, 128], F32)
    gm = P.sbuf("gm", [64, 3, 64], F32)
    smask = P.sbuf("smask", [8, 512], F32)
    P.dma("sp", modc[:], modc_d[:, :, :], writes=["modc"])
    P.dma("sp", wcv[:], wcv_d[:, :, :], writes=["wcv"])
    P.dma("sp", hp[:], hp_d[:, :], writes=["hp"])
    P.dma("sp", gnb[:], gn_d[0, :].partition_broadcast(64), writes=["gnb"])
    P.dma("sp", OH[:], oh_d[:, :, :], writes=["OH"])
    P.dma("sp", gm[:], mk_d.rearrange("k p f -> p k f"), writes=["gm"])
    P.dma("sp", smask[:], sm_d[:, :], writes=["smask"])
    for dc in range(8):
        P.dma("pool", win[:, dc, :], win_d[dc], writes=["win"])
    P.dma("pool", wo[:], wo_d.rearrange("c p f -> p c f"), writes=["wo"])
    P.ts("dve", modc[:, :, 0:1], modc[:, :, 0:1], 1.0, None, ALU.add, None, ["modc"], ["modc"])
    P.act(nea[:], hp[:, 0:1], AF.Exp, ["hp"], ["nea"])
    P.ts("dve", nea[:], nea[:], -1.0, None, ALU.mult, None, ["nea"], ["nea"])
    strictL, upperI, id64 = gm[:, 0, :], gm[:, 1, :], gm[:, 2, :]

    xin = [P.sbuf("xin%d" % i, [128, 512], F32) for i in range(2)]
    hT = P.sbuf("hT", [128, 8, 512], BF16)
    pre = [P.sbuf("pre%d" % i, [128, 515], F32) for i in range(2)]
    hist = P.sbuf("hist", [128, 16, 3], F32)
    cv = [P.sbuf("cv%d" % i, [128, 512], F32) for i in range(2)]
    sqt = P.sbuf("sqt", [128, 512], F32)
    rst = P.sbuf("rst", [128, 512], F32)
    vcT = P.sbuf("vcT", [128, 8, 512], BF16)
    qTn = P.sbuf("qTn", [128, 4, 512], BF16)
    kTn = P.sbuf("kTn", [128, 4, 512], BF16)
    qgT = P.sbuf("qgT", [128, 8, 512], BF16)
    ogT = P.sbuf("ogT", [128, 8, 512], BF16)
    Bf = P.sbuf("Bf", [8, 512], F32)
    gf = P.sbuf("gf", [8, 512], F32)
    Gf = P.sbuf("Gf", [8, 512], F32)
    eG = P.sbuf("eG", [8, 512], F32)
    eD = P.sbuf("eD", [8, 512], F32)
    Zt = P.sbuf("Zt", [8, 8, 8], F32)
    glb = P.sbuf("glb", [128, 8, 8], F32)
    tok = P.sbuf("tok", [64, 4, 8, 8], F32)
    ntb = P.sbuf("ntb", [64, 8, 8], F32)
    zs = [P.sbuf("zs%d" % i, [64, 1024], BF16) for i in range(2)]
    ktok = [P.sbuf("ktok%d" % i, [64, 4, 128], BF16) for i in range(2)]
    vtok = [P.sbuf("vtok%d" % i, [64, 8, 128], BF16) for i in range(2)]
    ycp = [P.sbuf("ycp%d" % i, [128, 512], F32) for i in range(2)]
    KKs = [P.sbuf("KKs%d" % i, [64, 64], F32) for i in range(2)]
    KQs = [P.sbuf("KQs%d" % i, [64, 64], F32) for i in range(2)]
    Sf = [P.sbuf("Sf%d" % i, [128, 128], F32) for i in range(8)]
    Sb = [P.sbuf("Sb%d" % i, [128, 128], BF16) for i in range(8)]
    NS = GDN_NSET

    def mkset(name, shape, dt, n=NS):
        return [P.sbuf("%s_%d" % (name, i), shape, dt) for i in range(n)]
    e1 = mkset("e1", [64, 64], F32); e2 = mkset("e2", [64, 64], F32)
    A_ = [mkset("Aa%d" % k, [64, 64], F32) for k in range(2)]
    B_ = [mkset("Bb%d" % k, [64, 64], F32) for k in range(2)]
    S_ = [mkset("Ss%d" % k, [64, 64], F32) for k in range(2)]
    AiT = mkset("AiT", [64, 64], BF16); TTb = mkset("TTb", [64, 64], BF16)
    kbg = mkset("kbg", [64, 128], BF16); vbt = mkset("vbt", [64, 128], BF16); kd = mkset("kd", [64, 128], BF16)
    uu = mkset("uu", [64, 128], F32); wTb = mkset("wTb", [128, 64], BF16); vnew = mkset("vnew", [64, 128], BF16)
    osq = mkset("osq", [64, 128], F32); ssq = mkset("ssq", [64, 1], F32); onn = mkset("onn", [64, 128], F32)
    og = mkset("og", [64, 128], BF16)

    for i in range(8):
        P.op("dve", lambda e, i=i: e.memset(Sf[i][:], 0.0), [], [("Sf", i)])
        P.op("pool", lambda e, i=i: e.memset(Sb[i][:], 0.0), [], [("Sb", i)])
    P.op("dve", lambda e: e.memset(hist[:], 0.0), [], ["hist"])

    rr = [0]

    def small():
        r = rr[0] % 5
        rr[0] += 1
        return ps[:, r, 0:128], ("ps", r)

    bigb = [0]

    def big():
        b = 6 + bigb[0] % 2
        bigb[0] += 1
        return ps[:, b, :], ("ps", b)

    def load_x(i):
        tb_, dc_ = i // 8, i % 8
        if tb_ < NTB:
            P.dma("sp", xin[i % 2][:], xT_d[dc_][:, tb_ * 512:(tb_ + 1) * 512], writes=[("xin", i % 2)])

    load_x(0)
    load_x(1)
    npre = 0
    for tb in range(NTB):
        tsl = slice(tb * 512, (tb + 1) * 512)
        for dc in range(8):
            sl = (tb * 8 + dc) % 2
            P.ts("dve" if dc % 2 == 0 else "pool", hT[:, dc, :], xin[sl][:], modc[:, dc, 0:1], modc[:, dc, 1:2],
                 ALU.mult, ALU.add, [("xin", sl), "modc"], [("hT", dc)])
            load_x(tb * 8 + dc + 2)
        hTk = [("hT", dc) for dc in range(8)]
        for ch in range(16):
            pb, pk = big()
            for dc in range(8):
                P.mm(pb, win[:, dc, ch * 128:(ch + 1) * 128], hT[:, dc, :], dc == 0, dc == 7, ["win", ("hT", dc)], [pk])
            pr = pre[npre % 2]
            prk = ("pre", npre % 2)
            cvt = cv[npre % 2]
            cvk = ("cv", npre % 2)
            npre += 1
            P.copy("pool", pr[:, 0:3], hist[:, ch, :], ["hist"], [prk])
            P.copy("act", pr[:, 3:515], pb, [pk], [prk])
            P.copy("pool", hist[:, ch, :], pr[:, 512:515], [prk], ["hist"])
            P.ts("dve", cvt[:], pr[:, 0:512], wcv[:, ch, 0:1], None, ALU.mult, None, [prk, "wcv"], [cvk])
            for i in range(1, 4):
                P.stt(cvt[:], pr[:, i:i + 512], wcv[:, ch, i:i + 1], cvt[:], ALU.mult, ALU.add, [prk, "wcv", cvk], [cvk])
            if ch >= 8:
                P.act(vcT[:, ch - 8, :], cvt[:], AF.Silu, [cvk], [("vcT", ch - 8)])
            else:
                P.act(cvt[:], cvt[:], AF.Silu, [cvk], [cvk])
                P.act(sqt[:], cvt[:], AF.Square, [cvk], ["sqt"])
                sb_, sk = big()
                P.mm(sb_, C["ones_f"][:], sqt[:], True, True, ["ones_f", "sqt"], [sk])
                P.ts("dve", rst[:], sb_, RMS_EPS, None, ALU.add, None, [sk], ["rst"])
                P.act(rst[:], rst[:], AF.Sqrt, ["rst"], ["rst"])
                P.op("dve", lambda e: e.reciprocal(out=rst[:], in_=rst[:]), ["rst"], ["rst"])
                if ch < 4:
                    P.stt(qTn[:, ch, :], cvt[:], 128.0 ** -0.5, rst[:], ALU.mult, ALU.mult, [cvk, "rst"], [("qTn", ch)])
                else:
                    P.tt("dve", kTn[:, ch - 4, :], cvt[:], rst[:], ALU.mult, [cvk, "rst"], [("kTn", ch - 4)])
        if stage == 1:
            return P.finalize()
        for v in range(2):
            pb, pk = big()
            for dc in range(8):
                P.mm(pb[0:8, :], win[:, dc, 3072 + 8 * v:3080 + 8 * v], hT[:, dc, :], dc == 0, dc == 7, ["win", ("hT", dc)], [pk])
            if v == 0:
                P.act(Bf[:], pb[0:8, :], AF.Sigmoid, [pk], ["Bf"])
            else:
                P.act(gf[:], pb[0:8, :], AF.Exp, [pk, "hp"], ["gf"], bias=hp[:, 1:2], scale=1.0)
        P.ts("dve", gf[:], gf[:], 1.0, None, ALU.add, None, ["gf"], ["gf"])
        P.act(gf[:], gf[:], AF.Ln, ["gf"], ["gf"])
        P.ts("dve", gf[:], gf[:], nea[:, 0:1], None, ALU.mult, None, ["gf", "nea"], ["gf"])
        P.op("dve", lambda e: e.tensor_tensor_scan(out=Gf[:], data0=smask[:], data1=gf[:], initial=0.0, op0=ALU.mult, op1=ALU.add),
             ["smask", "gf"], ["Gf"])
        P.act(eG[:], Gf[:], AF.Exp, ["Gf"], ["eG"])
        for c in range(8):
            P.ts("dve", eD[:, c * 64:(c + 1) * 64], Gf[:, c * 64:(c + 1) * 64], Gf[:, c * 64 + 63:c * 64 + 64], -1.0,
                 ALU.subtract, ALU.mult, ["Gf"], ["eD"])
            P.ts("pool", Zt[:, c, :], C["ident_f"][0:8, 0:8], eG[:, c * 64 + 63:c * 64 + 64], None, ALU.mult, None,
                 ["ident_f", "eG"], ["Zt"])
        P.act(eD[:], eD[:], AF.Exp, ["eD"], ["eD"])
        pb, pk = big()
        P.mm(pb[:, 0:64], C["ones_f"][0:8, :], Zt[:].rearrange("p a b -> p (a b)"), True, True, ["ones_f", "Zt"], [pk])
        P.copy("act", glb[:].rearrange("p a b -> p (a b)"), pb[:, 0:64], [pk], ["glb"])
        if stage == 2:
            return P.finalize()
        pb, pk = big()
        for kind, (src, sk) in enumerate([(Gf, "Gf"), (Bf, "Bf"), (eG, "eG"), (eD, "eD")]):
            for c in range(8):
                P.tr(pb[0:64, kind * 64 + c * 8:kind * 64 + c * 8 + 8], src[:, c * 64:(c + 1) * 64], C["ident_f"][0:8, 0:8],
                     [sk, "ident_f"], [pk], inc=(c == 7))
        P.copy("act", tok[:].rearrange("p k c h -> p (k c h)"), pb[0:64, 0:256], [pk], ["tok"])
        P.ts("dve", ntb[:], tok[:, 1, :, :], -1.0, None, ALU.mult, None, ["tok"], ["ntb"])
        for hv in range(8):
            pb, pk = big()
            P.mm(pb, OH[:, hv, :], eG[:], True, True, ["OH", "eG"], [pk])
            P.tt("dve", qgT[:, hv, :], qTn[:, hv // 2, :], pb, ALU.mult, [("qTn", hv // 2), pk], [("qgT", hv)])
        if stage == 3:
            return P.finalize()
        for c in range(8):
            csl = slice(c * 64, (c + 1) * 64)
            cb = c % 2
            for half in range(2):
                pb, pk = big()
                for dc in range(8):
                    P.mm(pb[0:64, :], hT[:, dc, csl], win[:, dc, 2048 + half * 512:2048 + (half + 1) * 512], dc == 0, dc == 7,
                         [("hT", dc), "win"], [pk])
                P.act(zs[cb][:, half * 512:(half + 1) * 512], pb[0:64, :], AF.Silu, [pk], [("zs", cb)])
            if stage == 35:
                return P.finalize()
            for hq in range(4):
                P.tr(psb5[0:64, hq * 128:(hq + 1) * 128], kTn[:, hq, csl], C["ident_b"][:], [("kTn", hq), "ident_b"], [("ps", 5)],
                     inc=(hq == 3))
            P.copy("act", ktok[cb][:].rearrange("p a b -> p (a b)"), psb5[0:64, 0:512], [("ps", 5)], [("ktok", cb)])
            if stage == 36:
                return P.finalize()
            for half in range(2):
                for hh in range(4):
                    hv = half * 4 + hh
                    P.tr(psb5[0:64, 512 + hh * 128:512 + (hh + 1) * 128], vcT[:, hv, csl], C["ident_b"][:], [("vcT", hv), "ident_b"],
                         [("ps", 5)], inc=(hh == 3))
                P.copy("act", vtok[cb][:, half * 4:(half + 1) * 4, :].rearrange("p a b -> p (a b)"), psb5[0:64, 512:1024], [("ps", 5)],
                       [("vtok", cb)])
            if stage == 4:
                return P.finalize()
            for hq in range(4):
                hb = hq % 2
                r, rk = small()
                P.mm(r[0:64, 0:64], kTn[:, hq, csl], kTn[:, hq, csl], True, True, [("kTn", hq)], [rk])
                P.copy("act", KKs[hb][:], r[0:64, 0:64], [rk], [("KKs", hb)])
                r, rk = small()
                P.mm(r[0:64, 0:64], kTn[:, hq, csl], qTn[:, hq, csl], True, True, [("kTn", hq), ("qTn", hq)], [rk])
                P.copy("act", KQs[hb][:], r[0:64, 0:64], [rk], [("KQs", hb)])
                for hv in (2 * hq, 2 * hq + 1):
                    s = hv % NS
                    tG = tok[:, 0, c, hv:hv + 1]
                    tB = tok[:, 1, c, hv:hv + 1]
                    teG = tok[:, 2, c, hv:hv + 1]
                    teD = tok[:, 3, c, hv:hv + 1]
                    nB = ntb[:, c, hv:hv + 1]
                    r, rk = small()
                    P.mm(r[0:64, 0:64], OH[:, hv, 0:64], Gf[:, csl], True, True, ["OH", "Gf"], [rk])
                    P.ts("dve", e1[s][:], r[0:64, 0:64], tG, 0.0, ALU.subtract, ALU.max, [rk, "tok"], [("e1", s)])
                    P.ts("dve", e2[s][:], r[0:64, 0:64], tG, 0.0, ALU.subtract, ALU.min, [rk, "tok"], [("e2", s)])
                    P.act(e1[s][:], e1[s][:], AF.Exp, [("e1", s)], [("e1", s)], scale=-1.0)
                    P.act(e2[s][:], e2[s][:], AF.Exp, [("e2", s)], [("e2", s)])
                    P.tt("pool", e1[s][:], e1[s][:], strictL, ALU.mult, [("e1", s), "gm"], [("e1", s)])
                    P.tt("pool", e2[s][:], e2[s][:], upperI, ALU.mult, [("e2", s), "gm"], [("e2", s)])
                    A0, B0 = A_[0][s], B_[0][s]
                    P.stt(A0[:], KKs[hb][:], nB, e1[s][:], ALU.mult, ALU.mult, [("KKs", hb), "ntb", ("e1", s)], [("A", 0, s)])
                    r, rk = small()
                    P.tr(r[0:64, 0:64], A0[:], C["ident_f"][0:64, 0:64], [("A", 0, s), "ident_f"], [rk])
                    P.copy("act", B0[:], r[0:64, 0:64], [rk], [("B", 0, s)])
                    P.tt("pool", AiT[s][:], KQs[hb][:], e2[s][:], ALU.mult, [("KQs", hb), ("e2", s)], [("AiT", s)])
                    if stage == 5:
                        return P.finalize()
                    P.tt("dve", S_[1][s][:], B0[:], id64, ALU.add, [("B", 0, s), "gm"], [("S", 1, s)])
                    for m in range(1, 6):
                        pa, pbk = (m - 1) % 2, m % 2
                        Ap, Bp, An, Bn = A_[pa][s], B_[pa][s], A_[pbk][s], B_[pbk][s]
                        Sc, Sn = S_[m % 2][s], S_[(m + 1) % 2][s]
                        r, rk = small()
                        P.mm(r[0:64, 0:64], Bp[:], Ap[:], True, True, [("B", pa, s), ("A", pa, s)], [rk])
                        P.copy("act", An[:], r[0:64, 0:64], [rk], [("A", pbk, s)])
                        if stage == 52:
                            return P.finalize()
                        if m < 5:
                            r2, rk2 = small()
                            P.mm(r2[0:64, 0:64], Ap[:], Bp[:], True, True, [("B", pa, s), ("A", pa, s)], [rk2])
                            P.copy("dve", Bn[:], r2[0:64, 0:64], [rk2], [("B", pbk, s)])
                        if stage == 53:
                            return P.finalize()
                        r3, rk3 = small()
                        P.mm(r3[0:64, 0:64], An[:], Sc[:], True, True, [("A", pbk, s), ("S", m % 2, s)], [rk3])
                        if m < 5:
                            P.tt("dve", Sn[:], Sc[:], r3[0:64, 0:64], ALU.add, [("S", m % 2, s), rk3], [("S", (m + 1) % 2, s)])
                        else:
                            P.tt("dve", TTb[s][:], Sc[:], r3[0:64, 0:64], ALU.add, [("S", m % 2, s), rk3], [("TTb", s)])
                        if stage == 54 or stage == 60 + m:
                            return P.finalize()
                    if stage == 6:
                        return P.finalize()
                    P.ts("pool", kbg[s][:], ktok[cb][:, hq, :], tB, teG, ALU.mult, ALU.mult, [("ktok", cb), "tok"], [("kbg", s)])
                    P.ts("pool", vbt[s][:], vtok[cb][:, hv, :], tB, None, ALU.mult, None, [("vtok", cb), "tok"], [("vbt", s)])
                    P.ts("pool", kd[s][:], ktok[cb][:, hq, :], teD, None, ALU.mult, None, [("ktok", cb), "tok"], [("kd", s)])
                    r, rk = small()
                    P.mm(r[0:64, :], TTb[s][:], vbt[s][:], True, True, [("TTb", s), ("vbt", s)], [rk])
                    P.copy("act", uu[s][:], r[0:64, :], [rk], [("uu", s)])
                    r, rk = small()
                    P.mm(r[:, 0:64], kbg[s][:], TTb[s][:], True, True, [("TTb", s), ("kbg", s)], [rk])
                    P.copy("act", wTb[s][:], r[:, 0:64], [rk], [("wTb", s)])
                    if stage == 7:
                        return P.finalize()
                    r, rk = small()
                    P.mm(r[0:64, :], wTb[s][:], Sb[hv][:], True, True, [("wTb", s), ("Sb", hv)], [rk])
                    P.tt("dve", vnew[s][:], uu[s][:], r[0:64, :], ALU.subtract, [("uu", s), rk], [("vnew", s)])
                    ro, rok = small()
                    P.mm(ro[0:64, :], qgT[:, hv, csl], Sb[hv][:], True, False, [("qgT", hv), ("Sb", hv)], [rok])
                    P.mm(ro[0:64, :], AiT[s][:], vnew[s][:], False, True, [("AiT", s), ("vnew", s)], [rok])
                    r, rk = small()
                    P.mm(r[:, :], kd[s][:], vnew[s][:], True, True, [("kd", s), ("vnew", s)], [rk])
                    P.stt(Sf[hv][:], Sf[hv][:], glb[:, c, hv:hv + 1], r[:, :], ALU.mult, ALU.add, [("Sf", hv), "glb", rk], [("Sf", hv)])
                    P.copy("act", Sb[hv][:], Sf[hv][:], [("Sf", hv)], [("Sb", hv)])
                    if stage == 8:
                        return P.finalize()
                    P.act(osq[s][:], ro[0:64, :], AF.Square, [rok], [("osq", s)])
                    P.copy("act", onn[s][:], ro[0:64, :], [rok], [("onn", s)])
                    P.op("dve", lambda e, s=s: e.reduce_sum(out=ssq[s][:], in_=osq[s][:], axis=AX.X), [("osq", s)], [("ssq", s)])
                    P.ts("dve", ssq[s][:], ssq[s][:], 1.0 / 128, RMS_EPS, ALU.mult, ALU.add, [("ssq", s)], [("ssq", s)])
                    P.act(ssq[s][:], ssq[s][:], AF.Sqrt, [("ssq", s)], [("ssq", s)])
                    P.op("dve", lambda e, s=s: e.reciprocal(out=ssq[s][:], in_=ssq[s][:]), [("ssq", s)], [("ssq", s)])
                    P.stt(onn[s][:], onn[s][:], ssq[s][:, 0:1], gnb[:], ALU.mult, ALU.mult, [("onn", s), ("ssq", s), "gnb"], [("onn", s)])
                    P.tt("pool", og[s][:], onn[s][:], zs[cb][:, hv * 128:(hv + 1) * 128], ALU.mult, [("onn", s), ("zs", cb)], [("og", s)])
                    r, rk = small()
                    rb = r.bitcast(BF16)
                    P.tr(rb[:, 0:64], og[s][:], C["ident_b"][0:64, 0:64], [("og", s), "ident_b"], [rk])
                    P.copy("act", ogT[:, hv, csl], rb[:, 0:64], [rk], [("ogT", hv)])
        for tq in range(4):
            t0 = tb * 512 + tq * 128
            for dh in range(2):
                pb, pk = big()
                for hv in range(8):
                    P.mm(pb, ogT[:, hv, tq * 128:(tq + 1) * 128], wo[:, hv, dh * 512:(dh + 1) * 512], hv == 0, hv == 7,
                         [("ogT", hv), "wo"], [pk])
                P.copy("act" if dh == 0 else "dve", ycp[dh][:], pb, [pk], [("ycp", dh)])
                P.dma("sp", y_d[t0:t0 + 128, dh * 512:(dh + 1) * 512], ycp[dh][:], reads=[("ycp", dh)], is_output=True)
    return P.finalize()


def gdn_consts_np():
    i = np.arange(64)
    oh = np.zeros((8, 8, 128), np.float32)
    for h in range(8):
        oh[h, h, :] = 1.0
    gmask = np.stack([(i[:, None] > i[None, :]), (i[:, None] <= i[None, :]), np.eye(64) > 0]).astype(np.float32)
    sm = np.ones((8, 512), np.float32)
    sm[:, ::64] = 0.0
    return oh, gmask, sm


def gdn_inputs(xb, md, w_in, w_conv, a_log, dt_bias, g_norm, w_o, g):
    im = {}
    im["xT"] = np.ascontiguousarray(xb.T.reshape(8, 128, SEQ))
    im["modc"] = np.ascontiguousarray(np.stack([pcol(md[1], 8), pcol(md[0], 8)], axis=-1))
    q0, k0, v0, z0, b0, a0 = 4 * g * 128, 1024 + 4 * g * 128, 2048 + 8 * g * 128, 4096 + 8 * g * 128, 6144 + 8 * g, 6160 + 8 * g
    cols = np.concatenate([np.arange(q0, q0 + 512), np.arange(k0, k0 + 512), np.arange(v0, v0 + 1024), np.arange(z0, z0 + 1024),
                           np.arange(b0, b0 + 8), np.arange(a0, a0 + 8)])
    im["win"] = np.ascontiguousarray(w_in[:, cols].reshape(8, 128, 3088))
    ccols = cols[:2048]
    im["wcv"] = np.ascontiguousarray(w_conv[:, ccols].reshape(4, 16, 128).transpose(2, 1, 0))
    im["hp"] = np.ascontiguousarray(np.stack([a_log[8 * g:8 * g + 8], dt_bias[8 * g:8 * g + 8]], axis=1))
    im["gn"] = np.ascontiguousarray(g_norm.reshape(1, 128))
    im["wo"] = np.ascontiguousarray(w_o.reshape(16, 128, 1024)[8 * g:8 * g + 8])
    oh, gmask, sm = gdn_consts_np()
    im["oh"], im["gmask"], im["smask"] = oh, gmask, sm
    c = host_consts()
    for k in ["ident_f", "ident_b", "ones_f"]:
        im[k] = c[k]
    return im


def _run(nc, ims):
    res = run_bass_kernel_spmd(nc, ims, core_ids=list(range(8)))
    return res.results


def kernel(x, c, ada_w, ada_b, ln_g, ln_b,
           mla_w_dqkv, mla_g_q, mla_g_kv, mla_w_uq, mla_w_ukv, mla_w_o,
           pool_w, pool_scale,
           gdn_w_in, gdn_w_conv, gdn_a_log, gdn_dt_bias, gdn_g_norm, gdn_w_o,
           moe_w_router, moe_b_router, moe_w_gate_up, moe_b_gate_up, moe_w_down, moe_b_down):
    f = lambda a: np.ascontiguousarray(np.asarray(a, dtype=np.float32))
    x = f(x).copy()
    mods = run_mods(f(c), f(ada_w), f(ada_b))
    cpost = host_consts_post()
    H = 2048
    for i in range(4):
        kind, j = i % 3, i // 3
        mdl = [mods[i, b].reshape(6, 1024) for b in range(4)]
        if kind == 0:
            nc = build_mla()
            ims = [mla_inputs(x[q // 2], mdl[q // 2], f(mla_w_dqkv[j]), f(mla_g_q[j]), f(mla_g_kv[j]), f(mla_w_uq[j]),
                              f(mla_w_ukv[j]), f(mla_w_o[j]), q % 2) for q in range(8)]
            res = _run(nc, ims)
            ys = [[res[2 * (q // 2)]["y"][(q % 2) * H:(q % 2 + 1) * H], res[2 * (q // 2) + 1]["y"][(q % 2) * H:(q % 2 + 1) * H]]
                  for q in range(8)]
        elif kind == 1:
            nc = build_pool()
            ims = [pool_inputs(x[q // 2], mdl[q // 2], f(pool_w[j]), f(pool_scale[j]), q % 2) for q in range(8)]
            res = _run(nc, ims)
            ys = [[np.ascontiguousarray(res[q]["yT"].reshape(1024, H).T)] for q in range(8)]
        else:
            nc = build_gdn()
            ims = [gdn_inputs(x[q // 2], mdl[q // 2], f(gdn_w_in[j]), f(gdn_w_conv[j]), f(gdn_a_log[j]), f(gdn_dt_bias[j]),
                              f(gdn_g_norm[j]), f(gdn_w_o[j]), q % 2) for q in range(8)]
            res = _run(nc, ims)
            ys = [[res[2 * (q // 2)]["y"][(q % 2) * H:(q % 2 + 1) * H], res[2 * (q // 2) + 1]["y"][(q % 2) * H:(q % 2 + 1) * H]]
                  for q in range(8)]
        del res
        nc = build_post(len(ys[0]))
        wr, br = f(moe_w_router[i]), f(moe_b_router[i])
        wgu, bgu, wd, bd = f(moe_w_gate_up[i]), f(moe_b_gate_up[i]), f(moe_w_down[i]), f(moe_b_down[i])
        shared = post_inputs(x[0, 0:H], [], mdl[0], f(ln_g[i]), f(ln_b[i]), wr, br, wgu, bgu, wd, bd)
        ims = []
        for q in range(8):
            b, g = q // 2, q % 2
            im = dict(cpost)
            im.update(shared)
            im["x"] = np.ascontiguousarray(x[b, g * H:(g + 1) * H])
            for k, y in enumerate(ys[q]):
                im["y%d" % k] = np.ascontiguousarray(y)
            md = mdl[b]
            im["modbc"] = np.ascontiguousarray(np.stack([md[2], md[4], md[3], md[5]]))
            ims.append(im)
        res = _run(nc, ims)
        for q in range(8):
            b, g = q // 2, q % 2
            x[b, g * H:(g + 1) * H] = res[q]["out"]
        del res
    return x
```
